# Optimizing a Trainium2 kernel written in Bass

```python
import jax, jax.numpy as jnp
from jax import lax
import numpy as np

D_MODEL = 1024
BATCH = 8
SEQ = 4096
DEPTH = 2

GRID_W = 64
CTX_LEN = 256

CONV_W = D_MODEL // 4
CONV_TAPS = 3
RET_W = D_MODEL // 2
RET_HEADS = 8
RET_DK = RET_W // RET_HEADS
RET_DV = RET_W // RET_HEADS
RET_CHUNK = 128
SGU_W = D_MODEL // 4
SGU_GROUPS = 4
SGU_CHUNK = 128
MIX_W = CONV_W + RET_W + SGU_W

OFF_CONV_B = 0
OFF_CONV_C = OFF_CONV_B + CONV_W
OFF_CONV_X = OFF_CONV_C + CONV_W
OFF_RET_Q = OFF_CONV_X + CONV_W
OFF_RET_K = OFF_RET_Q + RET_W
OFF_RET_V = OFF_RET_K + RET_W
OFF_RET_G = OFF_RET_V + RET_W
OFF_SGU_U = OFF_RET_G + RET_W
OFF_SGU_V = OFF_SGU_U + SGU_W
IN_COLS = OFF_SGU_V + SGU_W
IN_SPLITS = (OFF_CONV_C, OFF_CONV_X, OFF_RET_Q, OFF_RET_K, OFF_RET_V, OFF_RET_G, OFF_SGU_U, OFF_SGU_V)

N_GROUPS = 4
EXPERTS_PER_GROUP = 8
N_EXPERTS = N_GROUPS * EXPERTS_PER_GROUP
TOP_K_IN_GROUP = 2
EXPERT_HIDDEN = D_MODEL // 2
DISPATCH_BLOCK = 128

N_MOD = 6
LN_EPS = 1e-5
DEEPNORM_ALPHA = (2 * DEPTH) ** 0.25
DEEPNORM_BETA = (8 * DEPTH) ** -0.25

kernel_name = "hybrid_dit_conv_retention_sgu_hmoe"


def _standardize(x):
    xf = x.astype(jnp.float32)
    mu = jnp.mean(xf, axis=-1, keepdims=True)
    var = jnp.mean(jnp.square(xf - mu), axis=-1, keepdims=True)
    return (xf - mu) * lax.rsqrt(var + LN_EPS)


def layer_norm(x, gain, bias):
    return (_standardize(x) * gain + bias).astype(x.dtype)


def conv3(z, w, axis):
    pad = [(0, 0)] * z.ndim
    pad[axis] = (1, 1)
    zp = jnp.pad(z, pad)
    n = z.shape[axis]
    out = w[0] * lax.slice_in_dim(zp, 0, n, axis=axis)
    out = out + w[1] * lax.slice_in_dim(zp, 1, n + 1, axis=axis)
    return out + w[2] * lax.slice_in_dim(zp, 2, n + 2, axis=axis)


def short_conv_grid(z, w):
    bsz, length, cw = z.shape
    rows = length // GRID_W
    zg = z.reshape(bsz, rows, GRID_W, cw)
    half = cw // 2
    yh = conv3(zg[..., :half], w[:, :half], axis=2)
    yv = conv3(zg[..., half:], w[:, half:], axis=1)
    return jnp.concatenate([yh, yv], axis=-1).reshape(bsz, length, cw)


def retention_scan(q, k, v, log_gamma, s0):
    f32 = jnp.float32
    bsz, length = q.shape[:2]
    nc = length // RET_CHUNK
    qc = q.astype(f32).reshape(bsz, nc, RET_CHUNK, RET_HEADS, RET_DK)
    kc = k.astype(f32).reshape(bsz, nc, RET_CHUNK, RET_HEADS, RET_DK)
    vc = v.astype(f32).reshape(bsz, nc, RET_CHUNK, RET_HEADS, RET_DV)
    lg = log_gamma.astype(f32)
    pos = jnp.arange(RET_CHUNK, dtype=f32)
    rel = pos[:, None] - pos[None, :]
    intra_decay = jnp.where(rel >= 0, jnp.exp(lg[:, None, None] * jnp.maximum(rel, 0.0)), 0.0)
    scores = jnp.einsum('bnihd,bnjhd->bnhij', qc, kc) * intra_decay
    intra = jnp.einsum('bnhij,bnjhe->bnihe', scores, vc)
    k_decay = jnp.exp((RET_CHUNK - 1 - pos)[:, None] * lg)
    chunk_kv = jnp.einsum('bnjhd,jh,bnjhe->nbhde', kc, k_decay, vc)
    chunk_decay = jnp.exp(RET_CHUNK * lg)[None, :, None, None]

    def step(state, kv):
        return chunk_decay * state + kv, state

    s_final, s_prev = lax.scan(step, s0.astype(f32), chunk_kv)
    q_decay = jnp.exp((pos + 1.0)[:, None] * lg)
    cross = jnp.einsum('bnihd,ih,nbhde->bnihe', qc, q_decay, s_prev)
    o = (intra + cross).reshape(bsz, length, RET_HEADS, RET_DV)
    return o, s_final


def retention_final_state(k, v, log_gamma):
    f32 = jnp.float32
    length = k.shape[1]
    pos = jnp.arange(length, dtype=f32)
    w = jnp.exp((length - 1 - pos)[:, None] * log_gamma.astype(f32))
    return jnp.einsum('blhd,lh,blhe->bhde', k.astype(f32), w, v.astype(f32))


def spatial_gating(u, v, w_s, b_s):
    bsz, length, width = v.shape
    nc = length // SGU_CHUNK
    vn = _standardize(v).astype(v.dtype)
    vn = vn.reshape(bsz, nc, SGU_CHUNK, SGU_GROUPS, width // SGU_GROUPS)
    mixed = jnp.einsum('gpq,bnqgc->bnpgc', w_s, vn) + b_s.T[:, :, None]
    return u * mixed.reshape(bsz, length, width)


def hybrid_mixer(h, w_in, w_out, conv_w, lg_f, lg_b, sgu_w, sgu_b, s0_f, s0_b, on_grid):
    bsz, length, _ = h.shape
    p = h @ w_in
    cb, cc, cx, q, k, v, g, u, vv = jnp.split(p, IN_SPLITS, axis=-1)
    z = cc * cx
    conv = short_conv_grid(z, conv_w) if on_grid else conv3(z, conv_w, axis=1)
    ya = cb * conv
    q = q.reshape(bsz, length, RET_HEADS, RET_DK)
    k = k.reshape(bsz, length, RET_HEADS, RET_DK) * (RET_DK ** -0.5)
    v = v.reshape(bsz, length, RET_HEADS, RET_DV)
    o_f, s_f = retention_scan(q, k, v, lg_f, s0_f)
    o_b, s_b = retention_scan(q[:, ::-1], k[:, ::-1], v[:, ::-1], lg_b, s0_b)
    o = _standardize(o_f + o_b[:, ::-1]).astype(h.dtype)
    yb = jax.nn.silu(g) * o.reshape(bsz, length, RET_W)
    yc = spatial_gating(u, vv, sgu_w, sgu_b)
    y = jnp.concatenate([ya, yb, yc], axis=-1) @ w_out
    return y, s_f, s_b


def context_retention_states(hc, w_in, lg_f, lg_b):
    bsz, length, _ = hc.shape
    k = (hc @ w_in[:, OFF_RET_K:OFF_RET_V]).reshape(bsz, length, RET_HEADS, RET_DK) * (RET_DK ** -0.5)
    v = (hc @ w_in[:, OFF_RET_V:OFF_RET_G]).reshape(bsz, length, RET_HEADS, RET_DV)
    return retention_final_state(k, v, lg_f), retention_final_state(k[:, ::-1], v[:, ::-1], lg_b)


def grouped_experts(xf, expert, token, gate, w_gate, w_up, w_down):
    n_tok, d = xf.shape
    m = expert.shape[0]
    order = jnp.argsort(expert)
    se = expert[order]
    counts = jnp.bincount(expert, length=N_EXPERTS)
    starts = jnp.cumsum(counts) - counts
    pcounts = (counts + DISPATCH_BLOCK - 1) // DISPATCH_BLOCK * DISPATCH_BLOCK
    pends = jnp.cumsum(pcounts)
    pstarts = pends - pcounts
    dest = pstarts[se] + (jnp.arange(m) - starts[se])
    n_blocks = -(-m // DISPATCH_BLOCK) + N_EXPERTS
    n_slots = n_blocks * DISPATCH_BLOCK
    slot_tok = jnp.full((n_slots,), n_tok, dtype=jnp.int32).at[dest].set(token[order].astype(jnp.int32))
    slot_gate = jnp.zeros((n_slots,), xf.dtype).at[dest].set(gate[order])
    blk_e = jnp.minimum(jnp.searchsorted(pends, jnp.arange(n_blocks) * DISPATCH_BLOCK, side='right'), N_EXPERTS - 1)
    xpad = jnp.concatenate([xf, jnp.zeros((1, d), xf.dtype)], axis=0)
    xb = xpad[slot_tok].reshape(n_blocks, DISPATCH_BLOCK, d)

    def expert_block(args):
        xblk, e = args
        return (jax.nn.silu(xblk @ w_gate[e]) * (xblk @ w_up[e])) @ w_down[e]

    yb = lax.map(expert_block, (xb, blk_e)).reshape(n_slots, d)
    y = jax.ops.segment_sum(yb * slot_gate[:, None], slot_tok, num_segments=n_tok + 1)
    return y[:n_tok]


def hier_moe(h, rg_w, rg_b, re_w, re_b, w_gate, w_up, w_down):
    bsz, length, d = h.shape
    xf = h.reshape(bsz * length, d)
    n_tok = xf.shape[0]
    pg = jax.nn.softmax((xf @ rg_w).astype(jnp.float32) + rg_b.astype(jnp.float32), axis=-1)
    g_idx = jnp.argmax(pg, axis=-1)
    g_prob = jnp.max(pg, axis=-1, keepdims=True)
    le_all = jnp.einsum('nd,gde->nge', xf, re_w).astype(jnp.float32) + re_b.astype(jnp.float32)
    le = le_all[jnp.arange(n_tok), g_idx]
    pe = jax.nn.softmax(le, axis=-1)
    top_p, top_i = lax.top_k(pe, TOP_K_IN_GROUP)
    gate = g_prob * top_p / jnp.sum(top_p, axis=-1, keepdims=True)
    expert = g_idx[:, None] * EXPERTS_PER_GROUP + top_i
    token = jnp.broadcast_to(jnp.arange(n_tok)[:, None], expert.shape)
    y = grouped_experts(xf, expert.reshape(-1), token.reshape(-1), gate.reshape(-1).astype(xf.dtype),
                        w_gate, w_up, w_down)
    return y.reshape(bsz, length, d)


def setup_inputs(seed: int = 0) -> dict:
    key = jax.random.key(seed)
    ks = jax.random.split(key, 32)
    f32 = jnp.float32

    def nrm(k, shape, scale):
        return jax.random.normal(k, shape, f32) * scale

    gamma_logit = jnp.asarray(np.log(2.0 ** (5 + np.arange(RET_HEADS)) - 1.0), f32)
    d = D_MODEL
    return {
        'x': nrm(ks[0], (BATCH, SEQ, d), 1.0),
        'c': nrm(ks[1], (BATCH, d), 1.0),
        'ctx': nrm(ks[2], (BATCH, CTX_LEN, d), 1.0),
        'c_ctx': nrm(ks[3], (d,), 1.0),
        'w_ada': nrm(ks[4], (DEPTH, d, N_MOD * d), 0.5 * d ** -0.5),
        'b_ada': nrm(ks[5], (DEPTH, N_MOD * d), 0.02),
        'w_in': nrm(ks[6], (DEPTH, d, IN_COLS), d ** -0.5),
        'conv_w': nrm(ks[7], (DEPTH, CONV_TAPS, CONV_W), CONV_TAPS ** -0.5),
        'ret_decay_fwd': gamma_logit + nrm(ks[8], (DEPTH, RET_HEADS), 0.1),
        'ret_decay_bwd': gamma_logit + nrm(ks[9], (DEPTH, RET_HEADS), 0.1),
        'sgu_w': nrm(ks[10], (DEPTH, SGU_GROUPS, SGU_CHUNK, SGU_CHUNK), SGU_CHUNK ** -0.5),
        'sgu_b': 1.0 + nrm(ks[11], (DEPTH, SGU_GROUPS, SGU_CHUNK), 0.02),
        'w_out': nrm(ks[12], (DEPTH, MIX_W, d), DEEPNORM_BETA * MIX_W ** -0.5),
        'ln1_g': 1.0 + nrm(ks[13], (DEPTH, d), 0.02),
        'ln1_b': nrm(ks[14], (DEPTH, d), 0.02),
        'router_group_w': nrm(ks[15], (DEPTH, d, N_GROUPS), d ** -0.5),
        'router_group_b': nrm(ks[16], (DEPTH, N_GROUPS), 0.01),
        'router_expert_w': nrm(ks[17], (DEPTH, N_GROUPS, d, EXPERTS_PER_GROUP), d ** -0.5),
        'router_expert_b': nrm(ks[18], (DEPTH, N_GROUPS, EXPERTS_PER_GROUP), 0.01),
        'moe_w_gate': nrm(ks[19], (DEPTH, N_EXPERTS, d, EXPERT_HIDDEN), d ** -0.5),
        'moe_w_up': nrm(ks[20], (DEPTH, N_EXPERTS, d, EXPERT_HIDDEN), d ** -0.5),
        'moe_w_down': nrm(ks[21], (DEPTH, N_EXPERTS, EXPERT_HIDDEN, d), DEEPNORM_BETA * EXPERT_HIDDEN ** -0.5),
        'ln2_g': 1.0 + nrm(ks[22], (DEPTH, d), 0.02),
        'ln2_b': nrm(ks[23], (DEPTH, d), 0.02),
    }


def reference(x, c, ctx, c_ctx, w_ada, b_ada, w_in, conv_w, ret_decay_fwd, ret_decay_bwd, sgu_w, sgu_b,
              w_out, ln1_g, ln1_b, router_group_w, router_group_b, router_expert_w, router_expert_b,
              moe_w_gate, moe_w_up, moe_w_down, ln2_g, ln2_b):
    alpha = DEEPNORM_ALPHA
    bsz = x.shape[0]
    for l in range(DEPTH):
        last = l == DEPTH - 1
        mod = (jax.nn.silu(c) @ w_ada[l] + b_ada[l])[:, None, :]
        sh1, sc1, g1, sh2, sc2, g2 = jnp.split(mod, N_MOD, axis=-1)
        mod_c = jax.nn.silu(c_ctx) @ w_ada[l] + b_ada[l]
        csh1, csc1, cg1, csh2, csc2, cg2 = jnp.split(mod_c, N_MOD)
        lg_f = jax.nn.log_sigmoid(ret_decay_fwd[l].astype(jnp.float32))
        lg_b = jax.nn.log_sigmoid(ret_decay_bwd[l].astype(jnp.float32))

        hc = ctx * (1.0 + csc1) + csh1
        if last:
            s_f, s_b = context_retention_states(hc, w_in[l], lg_f, lg_b)
        else:
            s_zero = jnp.zeros((bsz, RET_HEADS, RET_DK, RET_DV), jnp.float32)
            yc, s_f, s_b = hybrid_mixer(hc, w_in[l], w_out[l], conv_w[l], lg_f, lg_b, sgu_w[l], sgu_b[l],
                                        s_zero, s_zero, on_grid=False)
            ctx = layer_norm(alpha * ctx + cg1 * yc, ln1_g[l], ln1_b[l])
            hm = ctx * (1.0 + csc2) + csh2
            ctx = layer_norm(alpha * ctx + cg2 * hier_moe(hm, router_group_w[l], router_group_b[l],
                                                          router_expert_w[l], router_expert_b[l],
                                                          moe_w_gate[l], moe_w_up[l], moe_w_down[l]),
                             ln2_g[l], ln2_b[l])

        h = x * (1.0 + sc1) + sh1
        y, _, _ = hybrid_mixer(h, w_in[l], w_out[l], conv_w[l], lg_f, lg_b, sgu_w[l], sgu_b[l],
                               s_f, s_b, on_grid=True)
        x = layer_norm(alpha * x + g1 * y, ln1_g[l], ln1_b[l])
        hm = x * (1.0 + sc2) + sh2
        x = layer_norm(alpha * x + g2 * hier_moe(hm, router_group_w[l], router_group_b[l],
                                                 router_expert_w[l], router_expert_b[l],
                                                 moe_w_gate[l], moe_w_up[l], moe_w_down[l]),
                       ln2_g[l], ln2_b[l])
    return x
```

```python
import contextlib
import numpy as np
import concourse.bass as bass
import concourse.mybir as mybir
from concourse.bass_utils import run_bass_kernel_spmd

F32 = mybir.dt.float32
BF16 = mybir.dt.bfloat16
AF = mybir.ActivationFunctionType
ALU = mybir.AluOpType
AX = mybir.AxisListType

D = 1024
SEQ = 4096
CTX = 256
DEPTH = 2
NCH = SEQ // 128
NCC = CTX // 128
IN_COLS = 3328
O_CB, O_CC, O_CX, O_Q, O_K, O_V, O_G, O_U, O_VV = 0, 256, 512, 768, 1280, 1792, 2304, 2816, 3072
NE = 32
EH = 512
ALPHA = float((2 * DEPTH) ** 0.25)
EPS = 1e-5
ZPAD = 64
import os
CUT = int(os.environ.get('MP_CUT', '0'))
SUB = int(os.environ.get('MP_SUB', '0'))


class Buf:
    __slots__ = ("name", "w", "r")

    def __init__(self, name):
        self.name = name
        self.w = None
        self.r = []


class Tl:
    def __init__(self, t, name):
        self.t = t
        self.b = Buf(name)

    def __getitem__(self, k):
        return self.t[k]


class Sched:
    CE = ("pe", "dve", "act", "pool")

    def __init__(self, nc, stack):
        self.nc = nc
        self.stack = stack
        self.eng = {"pe": nc.tensor, "dve": nc.vector, "act": nc.scalar, "pool": nc.gpsimd, "sp": nc.sync}
        self.sem = {e: stack.enter_context(nc.semaphore("s_" + e)) for e in self.CE}
        self.cnt = {e: 0 for e in self.CE}
        self.known = {e: {} for e in self.eng}
        self.ops = {e: [] for e in self.eng}
        self.dsem = {}
        self.dcnt = {}
        self.semname = {}
        self.nops = 0

    def _dma_sem(self, buf):
        k = id(buf)
        if k not in self.dsem:
            s = self.stack.enter_context(self.nc.semaphore("d%d" % len(self.dsem)))
            self.dsem[k] = s
            self.dcnt[k] = 0
        return k

    def op(self, e, fn, R=(), W=(), dma=None):
        deps = []
        for t in R:
            if t.b.w is not None:
                deps.append(t.b.w)
        dk = ("d", self._dma_sem(dma.b)) if dma is not None else None
        for t in W:
            if t.b.w is not None and not (dk is not None and t.b.w[0] == dk):
                deps.append(t.b.w)
            deps.extend(t.b.r)
        if dma is not None:
            k = dk[1]
            self.dcnt[k] += 16
            tok = (("d", k), self.dcnt[k])
            inc = (self.dsem[k], 16)
        else:
            self.cnt[e] += 1
            tok = (("c", e), self.cnt[e])
            inc = (self.sem[e], 1)
        waits = []
        kn = self.known[e]
        for (sk, v) in deps:
            if sk == ("c", "pe") and e == "pe" and dma is None:
                continue
            if kn.get(sk, 0) >= v:
                continue
            kn[sk] = v
            waits.append((self.sem[sk[1]] if sk[0] == "c" else self.dsem[sk[1]], v))
        self.ops[e].append((waits, fn, inc))
        for t in R:
            t.b.r.append(tok)
        for t in W:
            t.b.w = tok
            t.b.r = []
        self.nops += 1

    def drain(self):
        waits = []
        kn = self.known["sp"]
        for k, c in self.dcnt.items():
            if c and kn.get(("d", k), 0) < c:
                kn[("d", k)] = c
                waits.append((self.dsem[k], c))
        for e in self.CE:
            if self.cnt[e] and kn.get(("c", e), 0) < self.cnt[e]:
                kn[("c", e)] = self.cnt[e]
                waits.append((self.sem[e], self.cnt[e]))
        self.ops["sp"].append((waits, None, None))

    def flush(self):
        self.drain()
        ops = self.ops
        with self.nc.Block() as block:
            def emit(eng, lst):
                for waits, fn, inc in lst:
                    for s, v in waits:
                        eng.wait_ge(s, v)
                    if fn is not None:
                        fn(eng).then_inc(inc[0], inc[1])

            if ops["pe"]:
                @block.tensor
                def _(eng):
                    emit(eng, ops["pe"])
            if ops["dve"]:
                @block.vector
                def _(eng):
                    emit(eng, ops["dve"])
            if ops["act"]:
                @block.scalar
                def _(eng):
                    emit(eng, ops["act"])
            if ops["pool"]:
                @block.gpsimd
                def _(eng):
                    emit(eng, ops["pool"])
            if ops["sp"]:
                @block.sync
                def _(eng):
                    emit(eng, ops["sp"])
        self.ops = {e: [] for e in self.eng}


class KB:
    def __init__(self, debug=False, stop=None):
        self.debug = debug
        self.stop = stop
        self.nc = bass.Bass("TRN2", target_bir_lowering=False)
        self.root = contextlib.ExitStack()

    def dram_in(self, name, shape):
        return Tl(self.nc.dram_tensor(name, list(shape), F32, kind="ExternalInput").ap(), name)

    def dram_tmp(self, name, shape, out=False):
        kind = "ExternalOutput" if (out or self.debug) else "Internal"
        return Tl(self.nc.dram_tensor(name, list(shape), F32, kind=kind).ap(), name)

    def sb(self, stack, name, shape, dt=F32):
        self.uid = getattr(self, "uid", 0) + 1
        name = "sb%d_%s" % (self.uid, name)
        return Tl(stack.enter_context(self.nc.sbuf_tensor(name, list(shape), dt)), name)

    def mm(self, out, lhsT, rhs, start, stop, R, W):
        self.S.op("pe", lambda e: e.matmul(out, lhsT, rhs, start=start, stop=stop), R, W)

    def tr(self, out, in_, R, W):
        ident = self.ident[:, :]
        self.S.op("pe", lambda e: e.transpose(out, in_, ident), list(R) + [self.ident], W)

    def dma(self, q, out, in_, R, W, sem, **kw):
        self.S.op(q, lambda e: e.dma_start(out, in_, **kw), R, W, dma=sem)

    def act(self, out, in_, func, R, W, bias=None, scale=None, accum=None, eng="act"):
        kw = {}
        if bias is not None:
            kw["bias"] = bias
        if scale is not None:
            kw["scale"] = scale
        if accum is not None:
            kw["accum_out"] = accum
        self.S.op(eng, lambda e: e.activation(out, in_, func, **kw), R, W)

    def tt(self, eng, out, a, b, op, R, W):
        self.S.op(eng, lambda e: e.tensor_tensor(out, a, b, op), R, W)

    def ts(self, eng, out, a, s1, s2, op0, op1, R, W):
        if op1 is None:
            self.S.op(eng, lambda e: e.tensor_scalar(out, a, s1, None, op0), R, W)
        else:
            self.S.op(eng, lambda e: e.tensor_scalar(out, a, s1, s2, op0, op1), R, W)

    def stt(self, out, a, s, b, op0, op1, R, W):
        self.S.op("dve", lambda e: e.scalar_tensor_tensor(out, a, s, b, op0, op1), R, W)

    def cp(self, eng, out, in_, R, W):
        if eng == "act":
            self.S.op("act", lambda e: e.activation(out, in_, AF.Copy), R, W)
        else:
            self.S.op(eng, lambda e: e.tensor_copy(out, in_), R, W)

    def red(self, out, in_, op, R, W, axis=AX.X):
        self.S.op("dve", lambda e: e.tensor_reduce(out, in_, axis, op), R, W)

    def layernorm(self, r, gam, bet, out, sc):
        st, mv, rs = sc["st"], sc["mv"], sc["rs"]
        for hh in range(2):
            self.S.op("dve", lambda e, hh=hh: e.bn_stats(st[:, hh * 6:(hh + 1) * 6], r[:, hh * 512:(hh + 1) * 512]), [r], [st])
        self.S.op("dve", lambda e: e.bn_aggr(mv[:, 0:2], st[:, 0:12]), [st], [mv])
        self.act(rs[:, 0:1], mv[:, 1:2], AF.Ln, [mv, self.epsT], [rs], bias=self.epsT[:, 0:1], scale=1.0)
        self.act(rs[:, 1:2], rs[:, 0:1], AF.Exp, [rs], [rs], scale=-0.5)
        self.ts("dve", r[:, :], r[:, :], mv[:, 0:1], rs[:, 1:2], ALU.subtract, ALU.mult, [r, mv, rs], [r])
        self.tt("pool", r[:, :], r[:, :], gam[:, :], ALU.mult, [r, gam], [r])
        self.tt("dve", out[:, :], r[:, :], bet[:, :], ALU.add, [r, bet], [out])

    def build(self):
        nc = self.nc
        root = self.root
        S = self.S = Sched(nc, root)
        I = {}
        I["x"] = self.dram_in("x", [SEQ, D])
        I["ctx"] = self.dram_in("ctx", [CTX, D])
        I["cT"] = self.dram_in("cT", [128, 16])
        I["w_ada"] = self.dram_in("w_ada", [DEPTH, D, 6 * D])
        I["b_ada"] = self.dram_in("b_ada", [DEPTH, 6 * D])
        I["w_in"] = self.dram_in("w_in", [DEPTH, D, IN_COLS])
        I["conv_w"] = self.dram_in("conv_w", [DEPTH, 3, 256])
        I["dec_f"] = self.dram_in("dec_f", [DEPTH, 8])
        I["dec_b"] = self.dram_in("dec_b", [DEPTH, 8])
        I["sgu_w"] = self.dram_in("sgu_w", [DEPTH, 4, 128, 128])
        I["sgu_b"] = self.dram_in("sgu_b", [DEPTH, 4, 128])
        I["w_out"] = self.dram_in("w_out", [DEPTH, D, D])
        I["ln1_g"] = self.dram_in("ln1_g", [DEPTH, D])
        I["ln1_b"] = self.dram_in("ln1_b", [DEPTH, D])
        I["wr"] = self.dram_in("wr", [DEPTH, D, 36])
        I["br"] = self.dram_in("br", [DEPTH, 36])
        nes = 1 if (self.stop or "").startswith("mi") or self.stop == "mod" else NE
        I["wg"] = self.dram_in("wg", [DEPTH, nes, D, EH])
        I["wu"] = self.dram_in("wu", [DEPTH, nes, D, EH])
        I["wd"] = self.dram_in("wd", [DEPTH, nes, EH, D])
        I["ln2_g"] = self.dram_in("ln2_g", [DEPTH, D])
        I["ln2_b"] = self.dram_in("ln2_b", [DEPTH, D])
        I["consts"] = self.dram_in("consts", [128, 8 * 128])
        self.I = I
        out = self.out = self.dram_tmp("out", [SEQ, D], out=True)
        modd = self.modd = self.dram_tmp("modd", [DEPTH, 2, 2, 6 * D])
        def scr(name, n):
            t = self.dram_tmp(name, [n * 128, D])
            return [Tl(t.t[i * 128:(i + 1) * 128, :], "%s_%d" % (name, i)) for i in range(n)]
        self.x1 = [scr("x1_%d" % l, NCH) for l in range(DEPTH)]
        self.x2 = [scr("x2_%d" % l, NCH) for l in range(DEPTH - 1)]
        self.c1 = [scr("c1_%d" % l, NCC) for l in range(DEPTH - 1)]
        self.c2 = [scr("c2_%d" % l, NCC) for l in range(DEPTH - 1)]
        self.xin = [Tl(I["x"].t[i * 128:(i + 1) * 128, :], "xin%d" % i) for i in range(NCH)]
        self.cin = [Tl(I["ctx"].t[i * 128:(i + 1) * 128, :], "cin%d" % i) for i in range(NCC)]
        self.outt = [Tl(out.t[i * 128:(i + 1) * 128, :], "out%d" % i) for i in range(NCH)]

        self.cst = self.sb(root, "cst", [128, 8, 128])
        self.ident = Tl(self.cst.t[:, 0, :], "ident")
        self.ident.b = self.cst.b
        self.epsT = self.sb(root, "epsT", [128, 1])
        self.sT = self.sb(root, "sT", [128, 8, 2])
        self.ps = [Tl(root.enter_context(nc.psum_tensor("ps%d" % i, [128, 512], F32)), "ps%d" % i) for i in range(8)]
        self.dma("sp", self.cst[:, :, :], I["consts"].t.rearrange("p (a b) -> p a b", b=128), [I["consts"]], [self.cst], self.cst)
        S.op("dve", lambda e: e.memset(self.epsT[:, :], EPS), [], [self.epsT])

        self.phase_mod()
        if self.stop == "mod":
            S.flush()
            return nc
        for l in range(DEPTH):
            self.phase_mixer(l)
            if self.stop == "mix%d" % l or self.stop in ("mixA", "mixB", "mixC"):
                break
            self.phase_moe(l)
            if self.stop == "moe%d" % l:
                break
        S.flush()
        return nc

    def phase_mod(self):
        S, I, ps = self.S, self.I, self.ps
        with contextlib.ExitStack() as st:
            cTs = self.sb(st, "cTs", [128, 8, 2])
            wa = [self.sb(st, "wa%d" % i, [128, 3072]) for i in range(2)]
            msb = self.sb(st, "msb", [2, 6 * D])
            m1p = self.sb(st, "m1p", [2, 6 * D])
            bad = self.sb(st, "bad", [2, 6 * D])
            self.dma("sp", cTs[:, :, :], I["cT"].t.rearrange("p (k m) -> p k m", m=2), [I["cT"]], [cTs], cTs)
            self.act(self.sT[:, :, :], cTs[:, :, :], AF.Silu, [cTs], [self.sT])
            it = 0
            for l in range(DEPTH):
                self.dma("sp", bad[:, :], I["b_ada"].t[l].partition_broadcast(2), [I["b_ada"]], [bad], bad)
                for half in range(2):
                    for kc in range(8):
                        w = wa[it % 2]
                        it += 1
                        self.dma("sp", w[:, :], I["w_ada"].t[l, kc * 128:(kc + 1) * 128, half * 3072:(half + 1) * 3072],
                                 [I["w_ada"]], [w], w)
                        for cb in range(6):
                            self.mm(ps[cb][0:2, :], self.sT[:, kc, :], w[:, cb * 512:(cb + 1) * 512], kc == 0, kc == 7,
                                    [self.sT, w], [ps[cb]])
                    for cb in range(6):
                        c0 = half * 3072 + cb * 512
                        self.tt("dve", msb[:, c0:c0 + 512], ps[cb][0:2, :], bad[:, c0:c0 + 512], ALU.add, [ps[cb], bad], [msb])
                self.ts("dve", m1p[:, :], msb[:, :], 1.0, None, ALU.add, None, [msb], [m1p])
                self.dma("sp", self.modd.t[l, :, 0, :], msb[:, :], [msb], [self.modd], msb)
                self.dma("sp", self.modd.t[l, :, 1, :], m1p[:, :], [m1p], [self.modd], m1p)
            S.flush()

    def phase_mixer(self, l):
        S, I, ps, nc = self.S, self.I, self.ps, self.nc
        last = l == DEPTH - 1
        with contextlib.ExitStack() as st:
            sb = lambda name, shape, dt=F32: self.sb(st, name, shape, dt)
            win = sb("win", [128, 8, IN_COLS], BF16)
            wout = sb("wout", [128, 8, D], BF16)
            for kc in range(8):
                self.dma("pool", win[:, kc, :], I["w_in"].t[l, kc * 128:(kc + 1) * 128, :], [I["w_in"]], [win], win,
                         max_dma_last_dim=8192)
            for kc in range(8):
                self.dma("pool", wout[:, kc, :], I["w_out"].t[l, kc * 128:(kc + 1) * 128, :], [I["w_out"]], [wout], wout,
                         max_dma_last_dim=8192)
            g1 = sb("g1", [128, D])
            lng = sb("lng", [128, D])
            lnb = sb("lnb", [128, D])
            self.dma("sp", lng[:, :], I["ln1_g"].t[l].partition_broadcast(128), [I["ln1_g"]], [lng], lng)
            self.dma("sp", lnb[:, :], I["ln1_b"].t[l].partition_broadcast(128), [I["ln1_b"]], [lnb], lnb)
            modT = sb("modT", [128, 2, 2, 8])
            for m in range(2):
                self.dma("sp", modT[:, m, 0, :], self.modd.t[l, m, 0, 0:1024].rearrange("(k p) -> p k", p=128),
                         [self.modd], [modT], modT, allow_slow_non_contiguous=True)
                self.dma("sp", modT[:, m, 1, :], self.modd.t[l, m, 1, 1024:2048].rearrange("(k p) -> p k", p=128),
                         [self.modd], [modT], modT, allow_slow_non_contiguous=True)
            dec = sb("dec", [128, 16])
            decq = sb("decq", [128, 8])
            self.dma("sp", dec[:, 0:8], I["dec_f"].t[l].partition_broadcast(128), [I["dec_f"]], [dec], dec)
            self.dma("sp", dec[:, 8:16], I["dec_b"].t[l].partition_broadcast(128), [I["dec_b"]], [dec], dec)
            for hp in range(2):
                for di, nm in enumerate(("dec_f", "dec_b")):
                    src = I[nm].t[l].rearrange("(c two) -> two c", two=2)[hp]
                    self.dma("sp", decq[hp * 64:(hp + 1) * 64, di * 4:(di + 1) * 4], src.partition_broadcast(64),
                             [I[nm]], [decq], decq, allow_slow_non_contiguous=True)
            lg = sb("lg", [128, 16])
            lgq = sb("lgq", [128, 8])
            for (src, dst) in ((dec, lg), (decq, lgq)):
                self.act(dst[:, :], src[:, :], AF.Exp, [src], [dst], scale=-1.0)
                self.act(dst[:, :], dst[:, :], AF.Ln, [dst], [dst], bias=1.0, scale=1.0)
                self.ts("dve", dst[:, :], dst[:, :], -1.0, None, ALU.mult, None, [dst], [dst])
            cst = self.cst
            dcomb = sb("dcomb", [128, 8, 128])
            dtmp = sb("dtmp", [128, 128])
            for h in range(8):
                self.act(dtmp[:, :], cst[:, 1, :], AF.Exp, [cst, lg], [dtmp], scale=lg[:, h:h + 1])
                self.tt("dve", dcomb[:, h, :], dtmp[:, :], cst[:, 2, :], ALU.mult, [dtmp, cst], [dcomb])
                self.act(dtmp[:, :], cst[:, 3, :], AF.Exp, [cst, lg], [dtmp], scale=lg[:, 8 + h:9 + h])
                self.tt("dve", dtmp[:, :], dtmp[:, :], cst[:, 4, :], ALU.mult, [dtmp, cst], [dtmp])
                self.tt("dve", dcomb[:, h, :], dcomb[:, h, :], dtmp[:, :], ALU.add, [dtmp, dcomb], [dcomb])
            tq = sb("tq", [128, 2, 4, 128])
            for c in range(4):
                self.act(tq[:, 0, c, :], cst[:, 5, :], AF.Exp, [cst, lgq], [tq], scale=lgq[:, c:c + 1])
                self.act(tq[:, 1, c, :], cst[:, 6, :], AF.Exp, [cst, lgq], [tq], scale=lgq[:, 4 + c:5 + c])
            self.ts("dve", tq[:, :, :, :], tq[:, :, :, :], 0.125, None, ALU.mult, None, [tq], [tq])
            tqm = sb("tqm", [128, 3, 2, 4, 128])
            pm8 = sb("pm8", [128, 2])
            self.ts("dve", pm8[:, :], cst[:, 7, 2:4], 0.125, None, ALU.mult, None, [cst], [pm8])
            for hp in range(2):
                self.ts("dve", tqm[:, 0, hp, :, :], cst[:, 5, :].unsqueeze(1).to_broadcast([128, 4, 128]), 0.0, pm8[:, hp:hp + 1],
                        ALU.mult, ALU.add, [cst, pm8], [tqm])
                for var in range(2):
                    self.ts("dve", tqm[:, 1 + var, hp, :, :], tq[:, var, :, :], cst[:, 7, 2 + hp:3 + hp], None, ALU.mult, None,
                            [tq, cst], [tqm])
            kd = sb("kd", [128, 2, 8])
            self.act(kd[:, 0, :], lg[:, 0:8], AF.Exp, [lg, cst], [kd], scale=cst[:, 7, 0:1])
            self.act(kd[:, 1, :], lg[:, 8:16], AF.Exp, [lg, cst], [kd], scale=cst[:, 7, 1:2])
            cd = sb("cd", [128, 2, 4])
            self.act(cd[:, 0, :], lgq[:, 0:4], AF.Exp, [lgq], [cd], scale=128.0)
            self.act(cd[:, 1, :], lgq[:, 4:8], AF.Exp, [lgq], [cd], scale=128.0)
            wsn = sb("wsn", [128, 4, 128])
            wsT = sb("wsT", [128, 4, 128], BF16)
            bsT = sb("bsT", [128, 2, 128])
            self.dma("sp", wsn[:, :, :], I["sgu_w"].t[l].rearrange("g p q -> p g q"), [I["sgu_w"]], [wsn], wsn)
            for g in range(4):
                self.tr(ps[0][:, g * 128:(g + 1) * 128], wsn[:, g, :], [wsn], [ps[0]])
            self.cp("act", wsT[:, :, :], ps[0][:, :].rearrange("p (g q) -> p g q", q=128), [ps[0]], [wsT])
            for gp in range(2):
                for gi in range(2):
                    self.dma("sp", bsT[gi * 64:(gi + 1) * 64, gp, :], I["sgu_b"].t[l, 2 * gp + gi].partition_broadcast(64),
                             [I["sgu_b"]], [bsT], bsT)
            cw = sb("cw", [128, 2, 3])
            for c in range(2):
                for k in range(3):
                    self.dma("sp", cw[:, c, k:k + 1], I["conv_w"].t[l, k, c * 128:(c + 1) * 128].rearrange("(p o) -> p o", o=1),
                             [I["conv_w"]], [cw], cw)
            zall = sb("zall", [128, 2, SEQ + 2 * ZPAD], BF16)
            sball = sb("sball", [128, NCH, 4, 64], BF16)
            st32 = [sb("st32_%d" % d_, [128, 4, 64]) for d_ in range(2)]
            sfbf = sb("sfbf", [128, 4, 64], BF16)
            stmp = sb("stmp", [128, 4, 64])
            s0 = [sb("s0_%d" % d_, [128, 4, 64]) for d_ in range(2)]
            W = dict(win=win, wout=wout, g1=g1, lng=lng, lnb=lnb, modT=modT, dcomb=dcomb, tq=tq, tqm=tqm, kd=kd, cd=cd,
                     wsT=wsT, bsT=bsT, cw=cw, zall=zall, sball=sball, st32=st32, sfbf=sfbf, stmp=stmp)
            wk = {}
            wk["xt"] = [sb("xt%d" % i, [128, D]) for i in range(2)]
            wk["hT"] = [sb("hT%d" % i, [128, 8, 128], BF16) for i in range(2)]
            wk["cxs"] = sb("cxs", [128, 2, 128])
            wk["ktok"] = sb("ktok", [128, 512], BF16)
            wk["vbf"] = sb("vbf", [128, 512], BF16)
            wk["vd"] = sb("vd", [128, 512], BF16)
            wk["q8T"] = sb("q8T", [128, 4, 128])
            wk["qm"] = sb("qm", [128, 6, 4, 128], BF16)
            wk["kT"] = sb("kT", [128, 4, 128], BF16)
            wk["msk"] = sb("msk", [128, 8, 128], BF16)
            wk["sg"] = sb("sg", [128, 512])
            wk["osb"] = sb("osb", [128, 512])
            wk["osq"] = sb("osq", [128, 512])
            wk["gst"] = sb("gst", [128, 4, 8])
            wk["yb"] = sb("yb", [128, 512])
            wk["mixT"] = sb("mixT", [128, 8, 128], BF16)
            wk["vn"] = sb("vn", [128, 256], BF16)
            wk["vst"] = sb("vst", [128, 16])
            wk["uT"] = sb("uT", [128, 2, 128])
            wk["cbs"] = sb("cbs", [128, 2, 128])
            wk["cv"] = sb("cv", [128, 2, 128])
            wk["mx"] = sb("mx", [128, 2, 128])
            wk["r"] = sb("r", [128, D])
            wk["xo"] = [sb("xo%d" % i, [128, D]) for i in range(2)]
            wk["lnsc"] = dict(st=sb("lnst", [128, 12]), mv=sb("lnmv", [128, 2]), rs=sb("lnrs", [128, 2]))
            self.wk = wk
            self.W = W
            self.itx = 0
            zero = lambda eng, t: S.op(eng, lambda e: e.memset(t[:], 0.0), [], [t])
            src_c = self.cin if l == 0 else self.c2[l - 1]
            self.dma("sp", g1[:, :], self.modd.t[l, 1, 0, 2048:3072].partition_broadcast(128), [self.modd], [g1], g1)
            if self.stop == "mixA":
                S.flush()
                return
            zero("pool", zall)
            zero("dve", st32[0])
            zero("dve", st32[1])
            self.prepass(l, 1, src_c, NCC, fwd_final=last)
            if self.stop == "mixB":
                S.flush()
                return
            if not last:
                zero("dve", st32[0])
                self.mainpass(l, 1, src_c, self.c1[l], NCC, grid=False)
            if self.stop == "mixC":
                S.flush()
                return
            for d_ in range(2):
                self.cp("dve", s0[d_][:, :, :], st32[d_][:, :, :], [st32[d_]], [s0[d_]])
            if self.debug:
                self.dbg_states = self.dram_tmp("dbgst%d" % l, [2, 128, 256])
                for d_ in range(2):
                    self.dma("sp", self.dbg_states.t[d_], s0[d_][:, :, :].rearrange("p c e -> p (c e)"), [s0[d_]], [self.dbg_states], s0[d_])
            src_x = self.xin if l == 0 else self.x2[l - 1]
            self.dma("sp", g1[:, :], self.modd.t[l, 0, 0, 2048:3072].partition_broadcast(128), [self.modd], [g1], g1)
            zero("pool", zall)
            self.prepass(l, 0, src_x, NCH, fwd_final=False)
            self.cp("dve", st32[0][:, :, :], s0[0][:, :, :], [s0[0]], [st32[0]])
            self.mainpass(l, 0, src_x, self.x1[l], NCH, grid=True)
            S.flush()

    def load_hT(self, l, m, src, n):
        S, ps, wk, W = self.S, self.ps, self.wk, self.W
        i = self.itx
        self.itx += 1
        xt = wk["xt"][i % 2]
        hT = wk["hT"][i % 2]
        self.dma("sp", xt[:, :], src[n][:, :], [src[n]], [xt], xt)
        for kc in range(8):
            self.tr(ps[kc // 4][:, (kc % 4) * 128:(kc % 4 + 1) * 128], xt[:, kc * 128:(kc + 1) * 128], [xt], [ps[kc // 4]])
        modT = W["modT"]
        for kc in range(8):
            src_ps = ps[kc // 4][:, (kc % 4) * 128:(kc % 4 + 1) * 128]
            if kc % 2 == 0:
                self.act(hT[:, kc, :], src_ps, AF.Identity, [ps[kc // 4], modT], [hT],
                         bias=modT[:, m, 0, kc:kc + 1], scale=modT[:, m, 1, kc:kc + 1])
            else:
                self.ts("dve", hT[:, kc, :], src_ps, modT[:, m, 1, kc:kc + 1], modT[:, m, 0, kc:kc + 1], ALU.mult, ALU.add,
                        [ps[kc // 4], modT], [hT])
        return xt, hT

    def proj_fm(self, hT, bank, slot, col0):
        win = self.W["win"]
        for kc in range(8):
            self.mm(bank[:, slot * 128:(slot + 1) * 128], win[:, kc, col0:col0 + 128], hT[:, kc, :], kc == 0, kc == 7,
                    [win, hT], [bank])

    def proj_tm(self, hT, bank, col0, ncols):
        win = self.W["win"]
        for kc in range(8):
            self.mm(bank[:, 0:ncols], hT[:, kc, :], win[:, kc, col0:col0 + ncols], kc == 0, kc == 7, [win, hT], [bank])

    def state_update(self, d_, bank, ktok, vd, dst_bf, fwd_weight=None):
        S, W = self.S, self.W
        st = W["st32"][d_]
        stmp = W["stmp"]
        cd = W["cd"]
        for c in range(4):
            self.mm(bank[:, c * 128:(c + 1) * 128], ktok[:, c * 128:(c + 1) * 128], vd[:, c * 128:(c + 1) * 128], True, True,
                    [ktok, vd], [bank])
        bv = bank[:, :].rearrange("p (c x) -> p c x", x=128)
        if fwd_weight is None:
            self.tt("pool", stmp[:, :, :], st[:, :, :], cd[:, d_, :].unsqueeze(2).to_broadcast([128, 4, 64]), ALU.mult, [st, cd], [stmp])
            for hp in range(2):
                p0, p1 = hp * 64, hp * 64 + 64
                self.tt("dve", st[p0:p1, :, :], stmp[p0:p1, :, :], bv[p0:p1, :, hp * 64:hp * 64 + 64], ALU.add, [stmp, bank], [st])
        else:
            for hp in range(2):
                p0, p1 = hp * 64, hp * 64 + 64
                if fwd_weight == "one":
                    self.tt("dve", st[p0:p1, :, :], st[p0:p1, :, :], bv[p0:p1, :, hp * 64:hp * 64 + 64], ALU.add, [st, bank], [st])
                else:
                    self.tt("dve", stmp[p0:p1, :, :], bv[p0:p1, :, hp * 64:hp * 64 + 64],
                            cd[p0:p1, d_, :].unsqueeze(2).to_broadcast([64, 4, 64]), ALU.mult, [bank, cd], [stmp])
                    self.tt("dve", st[p0:p1, :, :], st[p0:p1, :, :], stmp[p0:p1, :, :], ALU.add, [st, stmp], [st])
        if dst_bf is not None:
            ap, tl = dst_bf
            self.cp("act", ap, st[:, :, :], [st], [tl])

    def prepass(self, l, m, src, N, fwd_final):
        S, ps, wk, W = self.S, self.ps, self.wk, self.W
        zall, sball, kd = W["zall"], W["sball"], W["kd"]
        st32 = W["st32"]
        self.cp("act", sball[:, N - 1, :, :], st32[1][:, :, :], [st32[1]], [sball])
        for n in range(N - 1, -1, -1):
            xt, hT = self.load_hT(l, m, src, n)
            for j, col in enumerate((O_CC, O_CC + 128, O_CX, O_CX + 128)):
                self.proj_fm(hT, ps[2], j, col)
            self.proj_tm(hT, ps[3], O_K, 512)
            self.proj_tm(hT, ps[4], O_V, 512)
            cxs = wk["cxs"]
            self.cp("act", cxs[:, :, :], ps[2][:, 256:512].rearrange("p (c t) -> p c t", t=128), [ps[2]], [cxs])
            self.tt("dve", zall[:, :, ZPAD + n * 128:ZPAD + (n + 1) * 128], ps[2][:, 0:256].rearrange("p (c t) -> p c t", t=128),
                    cxs[:, :, :], ALU.mult, [ps[2], cxs], [zall])
            ktok, vd = wk["ktok"], wk["vd"]
            self.cp("act", ktok[:, :], ps[3][:, :], [ps[3]], [ktok])
            self.tt("dve", vd[:, :].rearrange("p (h e) -> p h e", e=64), ps[4][:, :].rearrange("p (h e) -> p h e", e=64),
                    kd[:, 1, :].unsqueeze(2).to_broadcast([128, 8, 64]), ALU.mult, [ps[4], kd], [vd])
            dst = (sball[:, n - 1, :, :], sball) if n >= 1 else None
            self.state_update(1, ps[5], ktok, vd, dst)
            if fwd_final:
                vdf = wk["vbf"]
                self.tt("dve", vdf[:, :].rearrange("p (h e) -> p h e", e=64), ps[4][:, :].rearrange("p (h e) -> p h e", e=64),
                        kd[:, 0, :].unsqueeze(2).to_broadcast([128, 8, 64]), ALU.mult, [ps[4], kd], [vdf])
                assert N == 2
                self.state_update(0, ps[6], ktok, vdf, None, fwd_weight=("one" if n == N - 1 else "cd"))

    def mainpass(self, l, m, src, dst, N, grid):
        S, ps, wk, W = self.S, self.ps, self.wk, self.W
        win, wout = W["win"], W["wout"]
        zall, sball, kd, tq, dcomb = W["zall"], W["sball"], W["kd"], W["tq"], W["dcomb"]
        st32, sfbf = W["st32"], W["sfbf"]
        self.cp("act", sfbf[:, :, :], st32[0][:, :, :], [st32[0]], [sfbf])
        for n in range(N):
            xt, hT = self.load_hT(l, m, src, n)
            for j, col in enumerate((O_CB, O_CB + 128, O_U, O_U + 128)):
                self.proj_fm(hT, ps[2], j, col)
            for c in range(4):
                self.proj_fm(hT, ps[3], c, O_Q + c * 128)
            for c in range(4):
                self.proj_fm(hT, ps[4], c, O_K + c * 128)
            self.proj_tm(hT, ps[5], O_V, 512)
            self.proj_tm(hT, ps[6], O_G, 512)
            self.proj_tm(hT, ps[7], O_K, 512)
            self.proj_tm(hT, ps[0], O_VV, 256)
            if CUT == 1:
                return
            q8T, qm, kT = wk["q8T"], wk["qm"], wk["kT"]
            tqm = W["tqm"]
            cbs, uT = wk["cbs"], wk["uT"]
            self.cp("act", q8T[:, :, :], ps[3][:, :].rearrange("p (c t) -> p c t", t=128), [ps[3]], [q8T])
            self.cp("act", kT[:, :, :], ps[4][:, :].rearrange("p (c t) -> p c t", t=128), [ps[4]], [kT])
            self.cp("act", cbs[:, :, :], ps[2][:, 0:256].rearrange("p (c t) -> p c t", t=128), [ps[2]], [cbs])
            self.cp("act", uT[:, :, :], ps[2][:, 256:512].rearrange("p (c t) -> p c t", t=128), [ps[2]], [uT])
            for vh in range(6):
                self.tt("dve", qm[:, vh, :, :], q8T[:, :, :], tqm[:, vh // 2, vh % 2, :, :], ALU.mult, [q8T, tqm], [qm])
            ktok, vbf, vd, sg = wk["ktok"], wk["vbf"], wk["vd"], wk["sg"]
            self.cp("act", vbf[:, :], ps[5][:, :], [ps[5]], [vbf])
            self.tt("dve", vd[:, :].rearrange("p (h e) -> p h e", e=64), ps[5][:, :].rearrange("p (h e) -> p h e", e=64),
                    kd[:, 0, :].unsqueeze(2).to_broadcast([128, 8, 64]), ALU.mult, [ps[5], kd], [vd])
            self.cp("act", ktok[:, :], ps[7][:, :], [ps[7]], [ktok])
            if SUB == 3:
                return
            self.act(sg[:, :], ps[6][:, :], AF.Exp, [ps[6]], [sg], scale=-1.0)
            self.ts("dve", sg[:, :], sg[:, :], 1.0, None, ALU.add, None, [sg], [sg])
            S.op("dve", lambda e: e.reciprocal(sg[:, :], sg[:, :]), [sg], [sg])
            self.tt("dve", sg[:, :], sg[:, :], ps[6][:, :], ALU.mult, [sg, ps[6]], [sg])
            if CUT == 2:
                return
            vst, vn = wk["vst"], wk["vn"]
            S.op("dve", lambda e: e.bn_stats(vst[:, 0:6], ps[0][:, 0:256]), [ps[0]], [vst])
            S.op("dve", lambda e: e.bn_aggr(vst[:, 6:8], vst[:, 0:6]), [vst], [vst])
            self.act(vst[:, 8:9], vst[:, 7:8], AF.Ln, [vst, self.epsT], [vst], bias=self.epsT[:, 0:1], scale=1.0)
            self.act(vst[:, 9:10], vst[:, 8:9], AF.Exp, [vst], [vst], scale=-0.5)
            self.ts("dve", vn[:, :], ps[0][:, 0:256], vst[:, 6:7], vst[:, 9:10], ALU.subtract, ALU.mult, [ps[0], vst], [vn])
            if CUT == 3:
                return
            msk = wk["msk"]
            for h in range(8):
                c, hp = h // 2, h % 2
                bank = ps[h // 4]
                self.mm(bank[:, (h % 4) * 128:(h % 4 + 1) * 128], kT[:, c, :], qm[:, 0 + hp, c, :],
                        True, True, [kT, qm], [bank])
            for b2 in range(2):
                self.tt("dve", msk[:, b2 * 4:(b2 + 1) * 4, :], ps[b2][:, :].rearrange("p (h t) -> p h t", t=128),
                        dcomb[:, b2 * 4:(b2 + 1) * 4, :], ALU.mult, [ps[b2], dcomb], [msk])
            if CUT == 4:
                return
            for h in range(8):
                c, hp = h // 2, h % 2
                p0, p1 = hp * 64, hp * 64 + 64
                o_ap = ps[3][:, h * 64:(h + 1) * 64]
                self.mm(o_ap, msk[:, h, :], vbf[:, h * 64:(h + 1) * 64], True, False, [msk, vbf], [ps[3]])
                self.mm(o_ap, qm[:, 2 + hp, c, :], sfbf[:, c, :], False, False, [qm, sfbf], [ps[3]])
                self.mm(o_ap, qm[:, 4 + hp, c, :], sball[:, n, c, :], False, True, [qm, sball], [ps[3]])
            if CUT == 5:
                return
            self.state_update(0, ps[5], ktok, vd, (sfbf[:, :, :], sfbf))
            if CUT == 6:
                return
            osb, osq, gst, yb = wk["osb"], wk["osq"], wk["gst"], wk["yb"]
            self.cp("act", osb[:, :], ps[3][:, :], [ps[3]], [osb])
            self.tt("dve", osq[:, :], osb[:, :], osb[:, :], ALU.mult, [osb], [osq])
            self.red(gst[:, 0, :], osb[:, :].rearrange("p (h e) -> p h e", e=64), ALU.add, [osb], [gst])
            self.red(gst[:, 1, :], osq[:, :].rearrange("p (h e) -> p h e", e=64), ALU.add, [osq], [gst])
            self.ts("dve", gst[:, 0:2, :], gst[:, 0:2, :], 1.0 / 64, None, ALU.mult, None, [gst], [gst])
            self.tt("dve", gst[:, 2, :], gst[:, 0, :], gst[:, 0, :], ALU.mult, [gst], [gst])
            self.tt("dve", gst[:, 1, :], gst[:, 1, :], gst[:, 2, :], ALU.subtract, [gst], [gst])
            self.act(gst[:, 2, :], gst[:, 1, :], AF.Ln, [gst, self.epsT], [gst], bias=self.epsT[:, 0:1], scale=1.0)
            self.act(gst[:, 3, :], gst[:, 2, :], AF.Exp, [gst], [gst], scale=-0.5)
            o3 = osb[:, :].rearrange("p (h e) -> p h e", e=64)
            self.tt("dve", o3, o3, gst[:, 0, :].unsqueeze(2).to_broadcast([128, 8, 64]), ALU.subtract, [osb, gst], [osb])
            self.tt("dve", o3, o3, gst[:, 3, :].unsqueeze(2).to_broadcast([128, 8, 64]), ALU.mult, [osb, gst], [osb])
            self.tt("dve", yb[:, :], osb[:, :], sg[:, :], ALU.mult, [osb, sg], [yb])
            if CUT == 7:
                return
            mixT = wk["mixT"]
            for c in range(4):
                self.tr(ps[4][:, c * 128:(c + 1) * 128], yb[:, c * 128:(c + 1) * 128], [yb], [ps[4]])
            self.cp("act", mixT[:, 2:6, :], ps[4][:, :].rearrange("p (c t) -> p c t", t=128), [ps[4]], [mixT])
            if CUT == 8:
                return
            wsT, bsT = W["wsT"], W["bsT"]
            for g in range(4):
                gp, gi = g // 2, g % 2
                self.mm(ps[7][:, g * 128:(g + 1) * 128], vn[:, gp * 128:(gp + 1) * 128], wsT[:, g, :], True, True,
                        [vn, wsT], [ps[7]])
            mx = wk["mx"]
            for g in range(4):
                gp, gi = g // 2, g % 2
                self.tt("dve", mx[gi * 64:(gi + 1) * 64, gp, :], ps[7][gi * 64:(gi + 1) * 64, g * 128:(g + 1) * 128],
                        bsT[gi * 64:(gi + 1) * 64, gp, :], ALU.add, [ps[7], bsT], [mx])
            self.tt("dve", mixT[:, 6:8, :], mx[:, :, :], uT[:, :, :], ALU.mult, [mx, uT], [mixT])
            if CUT == 9:
                return
            cv, cw = wk["cv"], W["cw"]
            t0 = ZPAD + n * 128
            if grid:
                z0 = zall[:, 0, t0:t0 + 128].rearrange("p (r w) -> p r w", w=64)
                c0 = cv[:, 0, :].rearrange("p (r w) -> p r w", w=64)
                self.ts("dve", cv[:, 0, :], zall[:, 0, t0:t0 + 128], cw[:, 0, 1:2], None, ALU.mult, None, [zall, cw], [cv])
                self.stt(c0[:, :, 1:64], z0[:, :, 0:63], cw[:, 0, 0:1], c0[:, :, 1:64], ALU.mult, ALU.add, [zall, cw, cv], [cv])
                self.stt(c0[:, :, 0:63], z0[:, :, 1:64], cw[:, 0, 2:3], c0[:, :, 0:63], ALU.mult, ALU.add, [zall, cw, cv], [cv])
                self.ts("dve", cv[:, 1, :], zall[:, 1, t0:t0 + 128], cw[:, 1, 1:2], None, ALU.mult, None, [zall, cw], [cv])
                self.stt(cv[:, 1, :], zall[:, 1, t0 - 64:t0 + 64], cw[:, 1, 0:1], cv[:, 1, :], ALU.mult, ALU.add, [zall, cw, cv], [cv])
                self.stt(cv[:, 1, :], zall[:, 1, t0 + 64:t0 + 192], cw[:, 1, 2:3], cv[:, 1, :], ALU.mult, ALU.add, [zall, cw, cv], [cv])
            else:
                for ch in range(2):
                    self.ts("dve", cv[:, ch, :], zall[:, ch, t0:t0 + 128], cw[:, ch, 1:2], None, ALU.mult, None, [zall, cw], [cv])
                    self.stt(cv[:, ch, :], zall[:, ch, t0 - 1:t0 + 127], cw[:, ch, 0:1], cv[:, ch, :], ALU.mult, ALU.add, [zall, cw, cv], [cv])
                    self.stt(cv[:, ch, :], zall[:, ch, t0 + 1:t0 + 129], cw[:, ch, 2:3], cv[:, ch, :], ALU.mult, ALU.add, [zall, cw, cv], [cv])
            self.tt("dve", mixT[:, 0:2, :], cv[:, :, :], cbs[:, :, :], ALU.mult, [cv, cbs], [mixT])
            if CUT == 10:
                return
            for hh, bank in ((0, ps[6]), (1, ps[1])):
                for kc in range(8):
                    self.mm(bank[:, :], mixT[:, kc, :], wout[:, kc, hh * 512:(hh + 1) * 512], kc == 0, kc == 7, [mixT, wout], [bank])
            if CUT == 11:
                return
            r, g1 = wk["r"], W["g1"]
            for hh, bank in ((0, ps[6]), (1, ps[1])):
                self.tt("dve", r[:, hh * 512:(hh + 1) * 512], bank[:, :], g1[:, hh * 512:(hh + 1) * 512], ALU.mult, [bank, g1], [r])
            self.stt(r[:, :], xt[:, :], ALPHA, r[:, :], ALU.mult, ALU.add, [xt, r], [r])
            xo = wk["xo"][n % 2]
            self.layernorm(r, W["lng"], W["lnb"], xo, wk["lnsc"])
            self.dma("sp", dst[n][:, :], xo[:, :], [xo], [dst[n]], xo)

    def phase_moe(self, l):
        S, I, ps, nc = self.S, self.I, self.ps, self.nc
        last = l == DEPTH - 1
        tiles = []
        if not last:
            for n in range(NCC):
                tiles.append((1, self.c1[l][n], self.c2[l][n]))
        for n in range(NCH):
            tiles.append((0, self.x1[l][n], self.outt[n] if last else self.x2[l][n]))
        ngrp = 3
        per = -(-len(tiles) // ngrp)
        groups = [tiles[i * per:(i + 1) * per] for i in range(ngrp)]
        GMAX = per
        with contextlib.ExitStack() as st:
            sb = lambda name, shape, dt=F32: self.sb(st, name, shape, dt)
            g2 = [sb("g2_%d" % m, [128, D]) for m in range(2)]
            lng = sb("lng2", [128, D])
            lnb = sb("lnb2", [128, D])
            for m in range(2):
                self.dma("sp", g2[m][:, :], self.modd.t[l, m, 0, 5120:6144].partition_broadcast(128), [self.modd], [g2[m]], g2[m])
            self.dma("sp", lng[:, :], I["ln2_g"].t[l].partition_broadcast(128), [I["ln2_g"]], [lng], lng)
            self.dma("sp", lnb[:, :], I["ln2_b"].t[l].partition_broadcast(128), [I["ln2_b"]], [lnb], lnb)
            modT = sb("modT2", [128, 2, 2, 8])
            for m in range(2):
                self.dma("sp", modT[:, m, 0, :], self.modd.t[l, m, 0, 3072:4096].rearrange("(k p) -> p k", p=128),
                         [self.modd], [modT], modT, allow_slow_non_contiguous=True)
                self.dma("sp", modT[:, m, 1, :], self.modd.t[l, m, 1, 4096:5120].rearrange("(k p) -> p k", p=128),
                         [self.modd], [modT], modT, allow_slow_non_contiguous=True)
            wr = sb("wr", [128, 8, 36])
            brt = sb("brt", [128, 36])
            self.dma("sp", wr[:, :, :], I["wr"].t[l].rearrange("(k p) c -> p k c", p=128), [I["wr"]], [wr], wr)
            self.dma("sp", brt[:, :], I["br"].t[l].partition_broadcast(128), [I["br"]], [brt], brt)
            yacc = [sb("yacc%d" % i, [128, D]) for i in range(GMAX)]
            hmT = [sb("hmT%d" % i, [128, 8, 512], BF16) for i in range((GMAX + 3) // 4)]
            gate = [sb("gate%d" % i, [128, 32]) for i in range(GMAX)]
            hmf = sb("hmf", [128, 8, 128])
            wgs = [sb("wgs%d" % i, [128, 8, EH], BF16) for i in range(2)]
            wus = [sb("wus%d" % i, [128, 8, EH], BF16) for i in range(2)]
            wds = [sb("wds%d" % i, [128, 4, D], BF16) for i in range(2)]
            xt = [sb("mxt%d" % i, [128, D]) for i in range(2)]
            xo = [sb("mxo%d" % i, [128, D]) for i in range(2)]
            r = sb("mr", [128, D])
            sgs = [sb("sgs%d" % i, [128, 512]) for i in range(2)]
            hT = [sb("hhT%d" % i, [128, 4, 512], BF16) for i in range(2)]
            rt = sb("rt", [128, 128])
            lnsc = dict(st=sb("lnst2", [128, 12]), mv=sb("lnmv2", [128, 2]), rs=sb("lnrs2", [128, 2]))
            wcount = 0
            hcount = 0
            for grp in groups:
                G = len(grp)
                for ti, (m, src, dst) in enumerate(grp):
                    x_t = xt[ti % 2]
                    self.dma("sp", x_t[:, :], src[:, :], [src], [x_t], x_t)
                    for kc in range(8):
                        self.tr(ps[kc // 4][:, (kc % 4) * 128:(kc % 4 + 1) * 128], x_t[:, kc * 128:(kc + 1) * 128], [x_t], [ps[kc // 4]])
                    for kc in range(8):
                        src_ps = ps[kc // 4][:, (kc % 4) * 128:(kc % 4 + 1) * 128]
                        self.act(hmf[:, kc, :], src_ps, AF.Identity, [ps[kc // 4], modT], [hmf],
                                 bias=modT[:, m, 0, kc:kc + 1], scale=modT[:, m, 1, kc:kc + 1])
                    self.cp("dve", hmT[ti // 4][:, :, (ti % 4) * 128:(ti % 4 + 1) * 128], hmf[:, :, :], [hmf], [hmT[ti // 4]])
                    for kc in range(8):
                        self.mm(ps[2][:, 0:36], hmf[:, kc, :], wr[:, kc, :], kc == 0, kc == 7, [hmf, wr], [ps[2]])
                    self.tt("dve", rt[:, 0:36], ps[2][:, 0:36], brt[:, :], ALU.add, [ps[2], brt], [rt])
                    self.red(rt[:, 40:41], rt[:, 0:4], ALU.max, [rt], [rt])
                    self.ts("dve", rt[:, 41:42], rt[:, 40:41], -1.0, None, ALU.mult, None, [rt], [rt])
                    self.act(rt[:, 44:48], rt[:, 0:4], AF.Exp, [rt], [rt], bias=rt[:, 41:42], scale=1.0)
                    self.red(rt[:, 42:43], rt[:, 44:48], ALU.add, [rt], [rt])
                    S.op("dve", lambda e: e.reciprocal(rt[:, 43:44], rt[:, 42:43]), [rt], [rt])
                    self.ts("dve", rt[:, 48:52], rt[:, 0:4], rt[:, 40:41], None, ALU.is_equal, None, [rt], [rt])
                    self.tt("dve", rt[:, 64:96].rearrange("p (g e) -> p g e", e=8), rt[:, 4:36].rearrange("p (g e) -> p g e", e=8),
                            rt[:, 48:52].unsqueeze(2).to_broadcast([128, 4, 8]), ALU.mult, [rt], [rt])
                    self.red(rt[:, 96:104], rt[:, 64:96].rearrange("p (g e) -> p e g", e=8), ALU.add, [rt], [rt])
                    S.op("dve", lambda e: e.max(rt[:, 104:112], rt[:, 96:104]), [rt], [rt])
                    self.ts("dve", rt[:, 112:120], rt[:, 96:104], rt[:, 105:106], None, ALU.is_ge, None, [rt], [rt])
                    self.ts("dve", rt[:, 52:53], rt[:, 104:105], -1.0, None, ALU.mult, None, [rt], [rt])
                    self.act(rt[:, 120:128], rt[:, 96:104], AF.Exp, [rt], [rt], bias=rt[:, 52:53], scale=1.0)
                    self.tt("dve", rt[:, 120:128], rt[:, 120:128], rt[:, 112:120], ALU.mult, [rt], [rt])
                    self.red(rt[:, 53:54], rt[:, 120:128], ALU.add, [rt], [rt])
                    S.op("dve", lambda e: e.reciprocal(rt[:, 54:55], rt[:, 53:54]), [rt], [rt])
                    self.tt("dve", rt[:, 54:55], rt[:, 54:55], rt[:, 43:44], ALU.mult, [rt], [rt])
                    self.ts("dve", rt[:, 120:128], rt[:, 120:128], rt[:, 54:55], None, ALU.mult, None, [rt], [rt])
                    self.tt("dve", gate[ti][:, :].rearrange("p (g e) -> p g e", e=8),
                            rt[:, 48:52].unsqueeze(2).to_broadcast([128, 4, 8]),
                            rt[:, 120:128].unsqueeze(1).to_broadcast([128, 4, 8]), ALU.mult, [rt], [gate[ti]])
                mts = [list(range(i, min(i + 4, G))) for i in range(0, G, 4)]
                for e_ in range(NE):
                    wg, wu, wd = wgs[wcount % 2], wus[wcount % 2], wds[wcount % 2]
                    wcount += 1
                    for k4 in range(2):
                        self.dma("pool", wg[:, k4 * 4:(k4 + 1) * 4, :],
                                 I["wg"].t[l, e_, k4 * 512:(k4 + 1) * 512, :].rearrange("(k p) c -> p k c", p=128),
                                 [I["wg"]], [wg], wg)
                        self.dma("pool", wu[:, k4 * 4:(k4 + 1) * 4, :],
                                 I["wu"].t[l, e_, k4 * 512:(k4 + 1) * 512, :].rearrange("(k p) c -> p k c", p=128),
                                 [I["wu"]], [wu], wu)
                    self.dma("pool", wd[:, :, :], I["wd"].t[l, e_].rearrange("(k p) c -> p k c", p=128), [I["wd"]], [wd], wd)
                    for mt in mts:
                        nt = len(mt)
                        hTt = hT[hcount % 2]
                        hcount += 1
                        for hc in range(4):
                            pg, pu = ps[hc % 2], ps[2 + hc % 2]
                            for (bank, w_) in ((pg, wg), (pu, wu)):
                                for kc in range(8):
                                    self.mm(bank[:, 0:nt * 128], w_[:, kc, hc * 128:(hc + 1) * 128], hmT[mt[0] // 4][:, kc, 0:nt * 128],
                                            kc == 0, kc == 7, [w_, hmT[mt[0] // 4]], [bank])
                            sg_ = sgs[hc % 2]
                            self.act(sg_[:, 0:nt * 128], pg[:, 0:nt * 128], AF.Exp, [pg], [sg_], scale=-1.0)
                            self.ts("dve", sg_[:, 0:nt * 128], sg_[:, 0:nt * 128], 1.0, None, ALU.add, None, [sg_], [sg_])
                            S.op("dve", lambda e, sg_=sg_, nt=nt: e.reciprocal(sg_[:, 0:nt * 128], sg_[:, 0:nt * 128]), [sg_], [sg_])
                            self.tt("dve", sg_[:, 0:nt * 128], sg_[:, 0:nt * 128], pg[:, 0:nt * 128], ALU.mult, [sg_, pg], [sg_])
                            self.tt("dve", hTt[:, hc, 0:nt * 128], sg_[:, 0:nt * 128], pu[:, 0:nt * 128], ALU.mult, [sg_, pu], [hTt])
                        for j, ti in enumerate(mt):
                            for hh in range(2):
                                bank = ps[4 + 2 * (j % 2) + hh]
                                for hc in range(4):
                                    self.mm(bank[:, :], hTt[:, hc, j * 128:(j + 1) * 128], wd[:, hc, hh * 512:(hh + 1) * 512], hc == 0, hc == 3,
                                            [hTt, wd], [bank])
                                ya = yacc[ti][:, hh * 512:(hh + 1) * 512]
                                if e_ == 0:
                                    self.ts("dve", ya, bank[:, :], gate[ti][:, 0:1], None, ALU.mult, None, [bank, gate[ti]], [yacc[ti]])
                                else:
                                    self.stt(ya, bank[:, :], gate[ti][:, e_:e_ + 1], ya, ALU.mult, ALU.add, [bank, gate[ti], yacc[ti]], [yacc[ti]])
                for ti, (m, src, dst) in enumerate(grp):
                    x_t = xt[ti % 2]
                    self.dma("sp", x_t[:, :], src[:, :], [src], [x_t], x_t)
                    self.tt("pool", r[:, :], yacc[ti][:, :], g2[m][:, :], ALU.mult, [yacc[ti], g2[m]], [r])
                    self.stt(r[:, :], x_t[:, :], ALPHA, r[:, :], ALU.mult, ALU.add, [x_t, r], [r])
                    xo_ = xo[ti % 2]
                    self.layernorm(r, lng, lnb, xo_, lnsc)
                    self.dma("sp", dst[:, :], xo_[:, :], [xo_], [dst], xo_)
            S.flush()


def _consts():
    c = np.zeros((128, 8, 128), np.float32)
    j = np.arange(128)[:, None].astype(np.float32)
    i = np.arange(128)[None, :].astype(np.float32)
    c[:, 0, :] = np.eye(128, dtype=np.float32)
    c[:, 1, :] = np.maximum(i - j, 0)
    c[:, 2, :] = (i >= j)
    c[:, 3, :] = np.maximum(j - i, 0)
    c[:, 4, :] = (j >= i)
    c[:, 5, :] = i + 1.0
    c[:, 6, :] = 128.0 - i
    c[:, 7, 0] = 127.0 - j[:, 0]
    c[:, 7, 1] = j[:, 0]
    c[:64, 7, 2] = 1.0
    c[64:, 7, 3] = 1.0
    return c.reshape(128, 8 * 128)


def make_in_maps(inputs):
    f = lambda a: np.ascontiguousarray(np.asarray(a, dtype=np.float32))
    wr = np.concatenate([f(inputs["router_group_w"])] + [f(inputs["router_expert_w"])[:, g] for g in range(4)], axis=2)
    br = np.concatenate([f(inputs["router_group_b"]), f(inputs["router_expert_b"]).reshape(DEPTH, 32)], axis=1)
    shared = {
        "w_ada": f(inputs["w_ada"]), "b_ada": f(inputs["b_ada"]), "w_in": f(inputs["w_in"]), "conv_w": f(inputs["conv_w"]),
        "dec_f": f(inputs["ret_decay_fwd"]), "dec_b": f(inputs["ret_decay_bwd"]), "sgu_w": f(inputs["sgu_w"]),
        "sgu_b": f(inputs["sgu_b"]), "w_out": f(inputs["w_out"]), "ln1_g": f(inputs["ln1_g"]), "ln1_b": f(inputs["ln1_b"]),
        "wr": f(wr), "br": f(br), "wg": f(inputs["moe_w_gate"]), "wu": f(inputs["moe_w_up"]), "wd": f(inputs["moe_w_down"]),
        "ln2_g": f(inputs["ln2_g"]), "ln2_b": f(inputs["ln2_b"]), "consts": _consts(),
    }
    x, c, ctx, c_ctx = f(inputs["x"]), f(inputs["c"]), f(inputs["ctx"]), f(inputs["c_ctx"])
    maps = []
    for b in range(8):
        cT = np.stack([c[b].reshape(8, 128).T, c_ctx.reshape(8, 128).T], axis=2).reshape(128, 16)
        d = dict(shared)
        d.update({"x": x[b], "ctx": ctx[b], "cT": f(cT)})
        maps.append(d)
    return maps


def kernel(**inputs):
    nc = KB().build()
    maps = make_in_maps(inputs)
    res = run_bass_kernel_spmd(nc, maps, core_ids=list(range(8)))
    return np.stack([np.asarray(r["out"]) for r in res.results], axis=0).astype(np.float32)
```

```python
import contextlib
import numpy as np
import concourse.bass as bass
import concourse.mybir as mybir
from concourse.bass_utils import run_bass_kernel_spmd

F32 = mybir.dt.float32
BF16 = mybir.dt.bfloat16
AF = mybir.ActivationFunctionType
ALU = mybir.AluOpType
AX = mybir.AxisListType

D = 1024
SEQ = 4096
CTX = 256
DEPTH = 2
NCH = SEQ // 128
NCC = CTX // 128
IN_COLS = 3328
O_CB, O_CC, O_CX, O_Q, O_K, O_V, O_G, O_U, O_VV = 0, 256, 512, 768, 1280, 1792, 2304, 2816, 3072
NE = 32
EH = 512
ALPHA = float((2 * DEPTH) ** 0.25)
EPS = 1e-5
ZPAD = 64
import os
CUT = int(os.environ.get('MP_CUT', '0'))
SUB = int(os.environ.get('MP_SUB', '0'))
SIG = int(os.environ.get('MP_SIG', '1'))


class Buf:
    __slots__ = ("name", "w", "r")

    def __init__(self, name):
        self.name = name
        self.w = None
        self.r = []


class Tl:
    def __init__(self, t, name):
        self.t = t
        self.b = Buf(name)

    def __getitem__(self, k):
        return self.t[k]


class Sched:
    CE = ("pe", "dve", "act", "pool")

    def __init__(self, nc, stack):
        self.nc = nc
        self.stack = stack
        self.eng = {"pe": nc.tensor, "dve": nc.vector, "act": nc.scalar, "pool": nc.gpsimd, "sp": nc.sync}
        self.sem = {e: stack.enter_context(nc.semaphore("s_" + e)) for e in self.CE}
        self.cnt = {e: 0 for e in self.CE}
        self.known = {e: {} for e in self.eng}
        self.ops = {e: [] for e in self.eng}
        self.dsem = {}
        self.dcnt = {}
        self.semname = {}
        self.nops = 0

    def _dma_sem(self, buf):
        k = id(buf)
        if k not in self.dsem:
            s = self.stack.enter_context(self.nc.semaphore("d%d" % len(self.dsem)))
            self.dsem[k] = s
            self.dcnt[k] = 0
        return k

    def op(self, e, fn, R=(), W=(), dma=None):
        deps = []
        for t in R:
            if t.b.w is not None:
                deps.append(t.b.w)
        dk = ("d", self._dma_sem(dma.b)) if dma is not None else None
        for t in W:
            if t.b.w is not None and not (dk is not None and t.b.w[0] == dk):
                deps.append(t.b.w)
            deps.extend(t.b.r)
        if dma is not None:
            k = dk[1]
            self.dcnt[k] += 16
            tok = (("d", k), self.dcnt[k])
            inc = (self.dsem[k], 16)
        else:
            self.cnt[e] += 1
            tok = (("c", e), self.cnt[e])
            inc = (self.sem[e], 1)
        waits = []
        kn = self.known[e]
        for (sk, v) in deps:
            if sk == ("c", "pe") and e == "pe" and dma is None:
                continue
            if kn.get(sk, 0) >= v:
                continue
            kn[sk] = v
            waits.append((self.sem[sk[1]] if sk[0] == "c" else self.dsem[sk[1]], v))
        self.ops[e].append((waits, fn, inc))
        for t in R:
            t.b.r.append(tok)
        for t in W:
            t.b.w = tok
            t.b.r = []
        self.nops += 1

    def drain(self):
        waits = []
        kn = self.known["sp"]
        for k, c in self.dcnt.items():
            if c and kn.get(("d", k), 0) < c:
                kn[("d", k)] = c
                waits.append((self.dsem[k], c))
        for e in self.CE:
            if self.cnt[e] and kn.get(("c", e), 0) < self.cnt[e]:
                kn[("c", e)] = self.cnt[e]
                waits.append((self.sem[e], self.cnt[e]))
        self.ops["sp"].append((waits, None, None))

    def flush(self):
        self.drain()
        ops = self.ops
        with self.nc.Block() as block:
            def emit(eng, lst):
                for waits, fn, inc in lst:
                    for s, v in waits:
                        eng.wait_ge(s, v)
                    if fn is not None:
                        fn(eng).then_inc(inc[0], inc[1])

            if ops["pe"]:
                @block.tensor
                def _(eng):
                    emit(eng, ops["pe"])
            if ops["dve"]:
                @block.vector
                def _(eng):
                    emit(eng, ops["dve"])
            if ops["act"]:
                @block.scalar
                def _(eng):
                    emit(eng, ops["act"])
            if ops["pool"]:
                @block.gpsimd
                def _(eng):
                    emit(eng, ops["pool"])
            if ops["sp"]:
                @block.sync
                def _(eng):
                    emit(eng, ops["sp"])
        self.ops = {e: [] for e in self.eng}


class KB:
    def __init__(self, debug=False, stop=None):
        self.debug = debug
        self.stop = stop
        self.nc = bass.Bass("TRN2", target_bir_lowering=False)
        self.root = contextlib.ExitStack()

    def dram_in(self, name, shape):
        return Tl(self.nc.dram_tensor(name, list(shape), F32, kind="ExternalInput").ap(), name)

    def dram_tmp(self, name, shape, out=False):
        kind = "ExternalOutput" if (out or self.debug) else "Internal"
        return Tl(self.nc.dram_tensor(name, list(shape), F32, kind=kind).ap(), name)

    def sb(self, stack, name, shape, dt=F32):
        self.uid = getattr(self, "uid", 0) + 1
        name = "sb%d_%s" % (self.uid, name)
        return Tl(stack.enter_context(self.nc.sbuf_tensor(name, list(shape), dt)), name)

    def mm(self, out, lhsT, rhs, start, stop, R, W):
        self.S.op("pe", lambda e: e.matmul(out, lhsT, rhs, start=start, stop=stop), R, W)

    def tr(self, out, in_, R, W):
        ident = self.ident[:, :]
        self.S.op("pe", lambda e: e.transpose(out, in_, ident), list(R) + [self.ident], W)

    def dma(self, q, out, in_, R, W, sem, **kw):
        self.S.op(q, lambda e: e.dma_start(out, in_, **kw), R, W, dma=sem)

    def act(self, out, in_, func, R, W, bias=None, scale=None, accum=None, eng="act"):
        kw = {}
        if bias is not None:
            kw["bias"] = bias
        if scale is not None:
            kw["scale"] = scale
        if accum is not None:
            kw["accum_out"] = accum
        self.S.op(eng, lambda e: e.activation(out, in_, func, **kw), R, W)

    def sigmoid(self, out_ap, in_ap, R, W):
        if SIG:
            self.act(out_ap, in_ap, AF.Sigmoid, R, W)
            return
        self.act(out_ap, in_ap, AF.Exp, R, W, scale=-1.0)
        self.act(out_ap, out_ap, AF.Ln, W, W, bias=1.0, scale=1.0)
        self.act(out_ap, out_ap, AF.Exp, W, W, scale=-1.0)

    def tt(self, eng, out, a, b, op, R, W):
        self.S.op(eng, lambda e: e.tensor_tensor(out, a, b, op), R, W)

    def ts(self, eng, out, a, s1, s2, op0, op1, R, W):
        if op1 is None:
            self.S.op(eng, lambda e: e.tensor_scalar(out, a, s1, None, op0), R, W)
        else:
            self.S.op(eng, lambda e: e.tensor_scalar(out, a, s1, s2, op0, op1), R, W)

    def stt(self, out, a, s, b, op0, op1, R, W):
        self.S.op("dve", lambda e: e.scalar_tensor_tensor(out, a, s, b, op0, op1), R, W)

    def cp(self, eng, out, in_, R, W):
        if eng == "act":
            self.S.op("act", lambda e: e.activation(out, in_, AF.Copy), R, W)
        else:
            self.S.op(eng, lambda e: e.tensor_copy(out, in_), R, W)

    def red(self, out, in_, op, R, W, axis=AX.X):
        self.S.op("dve", lambda e: e.tensor_reduce(out, in_, axis, op), R, W)

    def layernorm(self, r, gam, bet, out, sc):
        st, mv, rs = sc["st"], sc["mv"], sc["rs"]
        for hh in range(2):
            self.S.op("dve", lambda e, hh=hh: e.bn_stats(st[:, hh * 6:(hh + 1) * 6], r[:, hh * 512:(hh + 1) * 512]), [r], [st])
        self.S.op("dve", lambda e: e.bn_aggr(mv[:, 0:2], st[:, 0:12]), [st], [mv])
        self.act(rs[:, 0:1], mv[:, 1:2], AF.Ln, [mv, self.epsT], [rs], bias=self.epsT[:, 0:1], scale=1.0)
        self.act(rs[:, 1:2], rs[:, 0:1], AF.Exp, [rs], [rs], scale=-0.5)
        self.ts("dve", r[:, :], r[:, :], mv[:, 0:1], rs[:, 1:2], ALU.subtract, ALU.mult, [r, mv, rs], [r])
        self.tt("pool", r[:, :], r[:, :], gam[:, :], ALU.mult, [r, gam], [r])
        self.tt("dve", out[:, :], r[:, :], bet[:, :], ALU.add, [r, bet], [out])

    def build(self):
        nc = self.nc
        root = self.root
        S = self.S = Sched(nc, root)
        I = {}
        I["x"] = self.dram_in("x", [SEQ, D])
        I["ctx"] = self.dram_in("ctx", [CTX, D])
        I["cT"] = self.dram_in("cT", [128, 16])
        I["w_ada"] = self.dram_in("w_ada", [DEPTH, D, 6 * D])
        I["b_ada"] = self.dram_in("b_ada", [DEPTH, 6 * D])
        I["w_in"] = self.dram_in("w_in", [DEPTH, D, IN_COLS])
        I["conv_w"] = self.dram_in("conv_w", [DEPTH, 3, 256])
        I["dec_f"] = self.dram_in("dec_f", [DEPTH, 8])
        I["dec_b"] = self.dram_in("dec_b", [DEPTH, 8])
        I["sgu_w"] = self.dram_in("sgu_w", [DEPTH, 4, 128, 128])
        I["sgu_b"] = self.dram_in("sgu_b", [DEPTH, 4, 128])
        I["w_out"] = self.dram_in("w_out", [DEPTH, D, D])
        I["ln1_g"] = self.dram_in("ln1_g", [DEPTH, D])
        I["ln1_b"] = self.dram_in("ln1_b", [DEPTH, D])
        I["wr"] = self.dram_in("wr", [DEPTH, D, 36])
        I["br"] = self.dram_in("br", [DEPTH, 36])
        nes = 1 if (self.stop or "").startswith("mi") or self.stop == "mod" else NE
        I["wg"] = self.dram_in("wg", [DEPTH, nes, D, EH])
        I["wu"] = self.dram_in("wu", [DEPTH, nes, D, EH])
        I["wd"] = self.dram_in("wd", [DEPTH, nes, EH, D])
        I["ln2_g"] = self.dram_in("ln2_g", [DEPTH, D])
        I["ln2_b"] = self.dram_in("ln2_b", [DEPTH, D])
        I["consts"] = self.dram_in("consts", [128, 8 * 128])
        self.I = I
        out = self.out = self.dram_tmp("out", [SEQ, D], out=True)
        modd = self.modd = self.dram_tmp("modd", [DEPTH, 2, 2, 6 * D])
        def scr(name, n):
            t = self.dram_tmp(name, [n * 128, D])
            return [Tl(t.t[i * 128:(i + 1) * 128, :], "%s_%d" % (name, i)) for i in range(n)]
        self.x1 = [scr("x1_%d" % l, NCH) for l in range(DEPTH)]
        self.x2 = [scr("x2_%d" % l, NCH) for l in range(DEPTH - 1)]
        self.c1 = [scr("c1_%d" % l, NCC) for l in range(DEPTH - 1)]
        self.c2 = [scr("c2_%d" % l, NCC) for l in range(DEPTH - 1)]
        self.xin = [Tl(I["x"].t[i * 128:(i + 1) * 128, :], "xin%d" % i) for i in range(NCH)]
        self.cin = [Tl(I["ctx"].t[i * 128:(i + 1) * 128, :], "cin%d" % i) for i in range(NCC)]
        self.outt = [Tl(out.t[i * 128:(i + 1) * 128, :], "out%d" % i) for i in range(NCH)]

        self.cst = self.sb(root, "cst", [128, 8, 128])
        self.ident = Tl(self.cst.t[:, 0, :], "ident")
        self.ident.b = self.cst.b
        self.epsT = self.sb(root, "epsT", [128, 1])
        self.sT = self.sb(root, "sT", [128, 8, 2])
        self.ps = [Tl(root.enter_context(nc.psum_tensor("ps%d" % i, [128, 512], F32)), "ps%d" % i) for i in range(8)]
        self.dma("sp", self.cst[:, :, :], I["consts"].t.rearrange("p (a b) -> p a b", b=128), [I["consts"]], [self.cst], self.cst)
        S.op("dve", lambda e: e.memset(self.epsT[:, :], EPS), [], [self.epsT])

        self.phase_mod()
        if self.stop == "mod":
            S.flush()
            return nc
        for l in range(DEPTH):
            self.phase_mixer(l)
            if self.stop == "mix%d" % l or self.stop in ("mixA", "mixB", "mixC"):
                break
            self.phase_moe(l)
            if self.stop == "moe%d" % l:
                break
        S.flush()
        return nc

    def phase_mod(self):
        S, I, ps = self.S, self.I, self.ps
        with contextlib.ExitStack() as st:
            cTs = self.sb(st, "cTs", [128, 8, 2])
            wa = [self.sb(st, "wa%d" % i, [128, 3072]) for i in range(2)]
            msb = self.sb(st, "msb", [2, 6 * D])
            m1p = self.sb(st, "m1p", [2, 6 * D])
            bad = self.sb(st, "bad", [2, 6 * D])
            self.dma("sp", cTs[:, :, :], I["cT"].t.rearrange("p (k m) -> p k m", m=2), [I["cT"]], [cTs], cTs)
            self.act(self.sT[:, :, :], cTs[:, :, :], AF.Silu, [cTs], [self.sT])
            it = 0
            for l in range(DEPTH):
                self.dma("sp", bad[:, :], I["b_ada"].t[l].partition_broadcast(2), [I["b_ada"]], [bad], bad)
                for half in range(2):
                    for kc in range(8):
                        w = wa[it % 2]
                        it += 1
                        self.dma("sp", w[:, :], I["w_ada"].t[l, kc * 128:(kc + 1) * 128, half * 3072:(half + 1) * 3072],
                                 [I["w_ada"]], [w], w)
                        for cb in range(6):
                            self.mm(ps[cb][0:2, :], self.sT[:, kc, :], w[:, cb * 512:(cb + 1) * 512], kc == 0, kc == 7,
                                    [self.sT, w], [ps[cb]])
                    for cb in range(6):
                        c0 = half * 3072 + cb * 512
                        self.tt("dve", msb[:, c0:c0 + 512], ps[cb][0:2, :], bad[:, c0:c0 + 512], ALU.add, [ps[cb], bad], [msb])
                self.ts("dve", m1p[:, :], msb[:, :], 1.0, None, ALU.add, None, [msb], [m1p])
                self.dma("sp", self.modd.t[l, :, 0, :], msb[:, :], [msb], [self.modd], msb)
                self.dma("sp", self.modd.t[l, :, 1, :], m1p[:, :], [m1p], [self.modd], m1p)
            S.flush()

    def phase_mixer(self, l):
        S, I, ps, nc = self.S, self.I, self.ps, self.nc
        last = l == DEPTH - 1
        with contextlib.ExitStack() as st:
            sb = lambda name, shape, dt=F32: self.sb(st, name, shape, dt)
            win = sb("win", [128, 8, IN_COLS], BF16)
            wout = sb("wout", [128, 8, D], BF16)
            for kc in range(8):
                self.dma("pool", win[:, kc, :], I["w_in"].t[l, kc * 128:(kc + 1) * 128, :], [I["w_in"]], [win], win,
                         max_dma_last_dim=8192)
            for kc in range(8):
                self.dma("pool", wout[:, kc, :], I["w_out"].t[l, kc * 128:(kc + 1) * 128, :], [I["w_out"]], [wout], wout,
                         max_dma_last_dim=8192)
            g1 = sb("g1", [128, D])
            lng = sb("lng", [128, D])
            lnb = sb("lnb", [128, D])
            self.dma("sp", lng[:, :], I["ln1_g"].t[l].partition_broadcast(128), [I["ln1_g"]], [lng], lng)
            self.dma("sp", lnb[:, :], I["ln1_b"].t[l].partition_broadcast(128), [I["ln1_b"]], [lnb], lnb)
            modT = sb("modT", [128, 2, 2, 8])
            for m in range(2):
                self.dma("sp", modT[:, m, 0, :], self.modd.t[l, m, 0, 0:1024].rearrange("(k p) -> p k", p=128),
                         [self.modd], [modT], modT, allow_slow_non_contiguous=True)
                self.dma("sp", modT[:, m, 1, :], self.modd.t[l, m, 1, 1024:2048].rearrange("(k p) -> p k", p=128),
                         [self.modd], [modT], modT, allow_slow_non_contiguous=True)
            dec = sb("dec", [128, 16])
            decq = sb("decq", [128, 8])
            self.dma("sp", dec[:, 0:8], I["dec_f"].t[l].partition_broadcast(128), [I["dec_f"]], [dec], dec)
            self.dma("sp", dec[:, 8:16], I["dec_b"].t[l].partition_broadcast(128), [I["dec_b"]], [dec], dec)
            for hp in range(2):
                for di, nm in enumerate(("dec_f", "dec_b")):
                    src = I[nm].t[l].rearrange("(c two) -> two c", two=2)[hp]
                    self.dma("sp", decq[hp * 64:(hp + 1) * 64, di * 4:(di + 1) * 4], src.partition_broadcast(64),
                             [I[nm]], [decq], decq, allow_slow_non_contiguous=True)
            lg = sb("lg", [128, 16])
            lgq = sb("lgq", [128, 8])
            for (src, dst) in ((dec, lg), (decq, lgq)):
                self.act(dst[:, :], src[:, :], AF.Exp, [src], [dst], scale=-1.0)
                self.act(dst[:, :], dst[:, :], AF.Ln, [dst], [dst], bias=1.0, scale=1.0)
                self.ts("dve", dst[:, :], dst[:, :], -1.0, None, ALU.mult, None, [dst], [dst])
            cst = self.cst
            dcomb = sb("dcomb", [128, 8, 128])
            dtmp = sb("dtmp", [128, 128])
            for h in range(8):
                self.act(dtmp[:, :], cst[:, 1, :], AF.Exp, [cst, lg], [dtmp], scale=lg[:, h:h + 1])
                self.tt("dve", dcomb[:, h, :], dtmp[:, :], cst[:, 2, :], ALU.mult, [dtmp, cst], [dcomb])
                self.act(dtmp[:, :], cst[:, 3, :], AF.Exp, [cst, lg], [dtmp], scale=lg[:, 8 + h:9 + h])
                self.tt("dve", dtmp[:, :], dtmp[:, :], cst[:, 4, :], ALU.mult, [dtmp, cst], [dtmp])
                self.tt("dve", dcomb[:, h, :], dcomb[:, h, :], dtmp[:, :], ALU.add, [dtmp, dcomb], [dcomb])
            tq = sb("tq", [128, 2, 4, 128])
            for c in range(4):
                self.act(tq[:, 0, c, :], cst[:, 5, :], AF.Exp, [cst, lgq], [tq], scale=lgq[:, c:c + 1])
                self.act(tq[:, 1, c, :], cst[:, 6, :], AF.Exp, [cst, lgq], [tq], scale=lgq[:, 4 + c:5 + c])
            self.ts("dve", tq[:, :, :, :], tq[:, :, :, :], 0.125, None, ALU.mult, None, [tq], [tq])
            tqm = sb("tqm", [128, 3, 2, 4, 128])
            pm8 = sb("pm8", [128, 2])
            self.ts("dve", pm8[:, :], cst[:, 7, 2:4], 0.125, None, ALU.mult, None, [cst], [pm8])
            for hp in range(2):
                self.ts("dve", tqm[:, 0, hp, :, :], cst[:, 5, :].unsqueeze(1).to_broadcast([128, 4, 128]), 0.0, pm8[:, hp:hp + 1],
                        ALU.mult, ALU.add, [cst, pm8], [tqm])
                for var in range(2):
                    self.ts("dve", tqm[:, 1 + var, hp, :, :], tq[:, var, :, :], cst[:, 7, 2 + hp:3 + hp], None, ALU.mult, None,
                            [tq, cst], [tqm])
            kd = sb("kd", [128, 2, 8])
            self.act(kd[:, 0, :], lg[:, 0:8], AF.Exp, [lg, cst], [kd], scale=cst[:, 7, 0:1])
            self.act(kd[:, 1, :], lg[:, 8:16], AF.Exp, [lg, cst], [kd], scale=cst[:, 7, 1:2])
            cd = sb("cd", [128, 2, 4])
            self.act(cd[:, 0, :], lgq[:, 0:4], AF.Exp, [lgq], [cd], scale=128.0)
            self.act(cd[:, 1, :], lgq[:, 4:8], AF.Exp, [lgq], [cd], scale=128.0)
            wsn = sb("wsn", [128, 4, 128])
            wsT = sb("wsT", [128, 4, 128], BF16)
            bsT = sb("bsT", [128, 2, 128])
            self.dma("sp", wsn[:, :, :], I["sgu_w"].t[l].rearrange("g p q -> p g q"), [I["sgu_w"]], [wsn], wsn)
            for g in range(4):
                self.tr(ps[0][:, g * 128:(g + 1) * 128], wsn[:, g, :], [wsn], [ps[0]])
            self.cp("act", wsT[:, :, :], ps[0][:, :].rearrange("p (g q) -> p g q", q=128), [ps[0]], [wsT])
            for gp in range(2):
                for gi in range(2):
                    self.dma("sp", bsT[gi * 64:(gi + 1) * 64, gp, :], I["sgu_b"].t[l, 2 * gp + gi].partition_broadcast(64),
                             [I["sgu_b"]], [bsT], bsT)
            cw = sb("cw", [128, 2, 3])
            for c in range(2):
                for k in range(3):
                    self.dma("sp", cw[:, c, k:k + 1], I["conv_w"].t[l, k, c * 128:(c + 1) * 128].rearrange("(p o) -> p o", o=1),
                             [I["conv_w"]], [cw], cw)
            zall = sb("zall", [128, 2, SEQ + 2 * ZPAD], BF16)
            sball = sb("sball", [128, NCH, 4, 64], BF16)
            st32 = [sb("st32_%d" % d_, [128, 4, 64]) for d_ in range(2)]
            sfbf = sb("sfbf", [128, 4, 64], BF16)
            stmp = sb("stmp", [128, 4, 64])
            s0 = [sb("s0_%d" % d_, [128, 4, 64]) for d_ in range(2)]
            W = dict(win=win, wout=wout, g1=g1, lng=lng, lnb=lnb, modT=modT, dcomb=dcomb, tq=tq, tqm=tqm, kd=kd, cd=cd,
                     wsT=wsT, bsT=bsT, cw=cw, zall=zall, sball=sball, st32=st32, sfbf=sfbf, stmp=stmp)
            wk = {}
            wk["xt"] = [sb("xt%d" % i, [128, D]) for i in range(2)]
            wk["hT"] = [sb("hT%d" % i, [128, 8, 128], BF16) for i in range(2)]
            wk["cxs"] = sb("cxs", [128, 2, 128])
            wk["ktok"] = sb("ktok", [128, 512], BF16)
            wk["vbf"] = sb("vbf", [128, 512], BF16)
            wk["vd"] = sb("vd", [128, 512], BF16)
            wk["q8T"] = sb("q8T", [128, 4, 128])
            wk["qm"] = sb("qm", [128, 6, 4, 128], BF16)
            wk["kT"] = sb("kT", [128, 4, 128], BF16)
            wk["msk"] = sb("msk", [128, 8, 128], BF16)
            wk["sg"] = sb("sg", [128, 512])
            wk["osb"] = sb("osb", [128, 512])
            wk["osq"] = sb("osq", [128, 512])
            wk["gst"] = sb("gst", [128, 4, 8])
            wk["yb"] = sb("yb", [128, 512])
            wk["mixT"] = sb("mixT", [128, 8, 128], BF16)
            wk["vn"] = sb("vn", [128, 256], BF16)
            wk["vst"] = sb("vst", [128, 16])
            wk["uT"] = sb("uT", [128, 2, 128])
            wk["cbs"] = sb("cbs", [128, 2, 128])
            wk["cv"] = sb("cv", [128, 2, 128])
            wk["mx"] = sb("mx", [128, 2, 128])
            wk["r"] = sb("r", [128, D])
            wk["xo"] = [sb("xo%d" % i, [128, D]) for i in range(2)]
            wk["lnsc"] = dict(st=sb("lnst", [128, 12]), mv=sb("lnmv", [128, 2]), rs=sb("lnrs", [128, 2]))
            self.wk = wk
            self.W = W
            self.itx = 0
            zero = lambda eng, t: S.op(eng, lambda e: e.memset(t[:], 0.0), [], [t])
            src_c = self.cin if l == 0 else self.c2[l - 1]
            self.dma("sp", g1[:, :], self.modd.t[l, 1, 0, 2048:3072].partition_broadcast(128), [self.modd], [g1], g1)
            if self.stop == "mixA":
                S.flush()
                return
            zero("pool", zall)
            zero("dve", st32[0])
            zero("dve", st32[1])
            self.prepass(l, 1, src_c, NCC, fwd_final=last)
            if self.stop == "mixB":
                S.flush()
                return
            if not last:
                zero("dve", st32[0])
                self.mainpass(l, 1, src_c, self.c1[l], NCC, grid=False)
            if self.stop == "mixC":
                S.flush()
                return
            for d_ in range(2):
                self.cp("dve", s0[d_][:, :, :], st32[d_][:, :, :], [st32[d_]], [s0[d_]])
            if self.debug:
                self.dbg_states = self.dram_tmp("dbgst%d" % l, [2, 128, 256])
                for d_ in range(2):
                    self.dma("sp", self.dbg_states.t[d_], s0[d_][:, :, :].rearrange("p c e -> p (c e)"), [s0[d_]], [self.dbg_states], s0[d_])
            src_x = self.xin if l == 0 else self.x2[l - 1]
            self.dma("sp", g1[:, :], self.modd.t[l, 0, 0, 2048:3072].partition_broadcast(128), [self.modd], [g1], g1)
            zero("pool", zall)
            self.prepass(l, 0, src_x, NCH, fwd_final=False)
            self.cp("dve", st32[0][:, :, :], s0[0][:, :, :], [s0[0]], [st32[0]])
            self.mainpass(l, 0, src_x, self.x1[l], NCH, grid=True)
            S.flush()

    def load_hT(self, l, m, src, n):
        S, ps, wk, W = self.S, self.ps, self.wk, self.W
        i = self.itx
        self.itx += 1
        xt = wk["xt"][i % 2]
        hT = wk["hT"][i % 2]
        self.dma("sp", xt[:, :], src[n][:, :], [src[n]], [xt], xt)
        for kc in range(8):
            self.tr(ps[kc // 4][:, (kc % 4) * 128:(kc % 4 + 1) * 128], xt[:, kc * 128:(kc + 1) * 128], [xt], [ps[kc // 4]])
        modT = W["modT"]
        for kc in range(8):
            src_ps = ps[kc // 4][:, (kc % 4) * 128:(kc % 4 + 1) * 128]
            if kc % 2 == 0:
                self.act(hT[:, kc, :], src_ps, AF.Identity, [ps[kc // 4], modT], [hT],
                         bias=modT[:, m, 0, kc:kc + 1], scale=modT[:, m, 1, kc:kc + 1])
            else:
                self.ts("dve", hT[:, kc, :], src_ps, modT[:, m, 1, kc:kc + 1], modT[:, m, 0, kc:kc + 1], ALU.mult, ALU.add,
                        [ps[kc // 4], modT], [hT])
        return xt, hT

    def proj_fm(self, hT, bank, slot, col0):
        win = self.W["win"]
        for kc in range(8):
            self.mm(bank[:, slot * 128:(slot + 1) * 128], win[:, kc, col0:col0 + 128], hT[:, kc, :], kc == 0, kc == 7,
                    [win, hT], [bank])

    def proj_tm(self, hT, bank, col0, ncols):
        win = self.W["win"]
        for kc in range(8):
            self.mm(bank[:, 0:ncols], hT[:, kc, :], win[:, kc, col0:col0 + ncols], kc == 0, kc == 7, [win, hT], [bank])

    def state_update(self, d_, bank, ktok, vd, dst_bf, fwd_weight=None):
        S, W = self.S, self.W
        st = W["st32"][d_]
        stmp = W["stmp"]
        cd = W["cd"]
        for c in range(4):
            self.mm(bank[:, c * 128:(c + 1) * 128], ktok[:, c * 128:(c + 1) * 128], vd[:, c * 128:(c + 1) * 128], True, True,
                    [ktok, vd], [bank])
        bv = bank[:, :].rearrange("p (c x) -> p c x", x=128)
        if fwd_weight is None:
            self.tt("pool", stmp[:, :, :], st[:, :, :], cd[:, d_, :].unsqueeze(2).to_broadcast([128, 4, 64]), ALU.mult, [st, cd], [stmp])
            for hp in range(2):
                p0, p1 = hp * 64, hp * 64 + 64
                self.tt("dve", st[p0:p1, :, :], stmp[p0:p1, :, :], bv[p0:p1, :, hp * 64:hp * 64 + 64], ALU.add, [stmp, bank], [st])
        else:
            for hp in range(2):
                p0, p1 = hp * 64, hp * 64 + 64
                if fwd_weight == "one":
                    self.tt("dve", st[p0:p1, :, :], st[p0:p1, :, :], bv[p0:p1, :, hp * 64:hp * 64 + 64], ALU.add, [st, bank], [st])
                else:
                    self.tt("dve", stmp[p0:p1, :, :], bv[p0:p1, :, hp * 64:hp * 64 + 64],
                            cd[p0:p1, d_, :].unsqueeze(2).to_broadcast([64, 4, 64]), ALU.mult, [bank, cd], [stmp])
                    self.tt("dve", st[p0:p1, :, :], st[p0:p1, :, :], stmp[p0:p1, :, :], ALU.add, [st, stmp], [st])
        if dst_bf is not None:
            ap, tl = dst_bf
            self.cp("act", ap, st[:, :, :], [st], [tl])

    def prepass(self, l, m, src, N, fwd_final):
        S, ps, wk, W = self.S, self.ps, self.wk, self.W
        zall, sball, kd = W["zall"], W["sball"], W["kd"]
        st32 = W["st32"]
        self.cp("act", sball[:, N - 1, :, :], st32[1][:, :, :], [st32[1]], [sball])
        for n in range(N - 1, -1, -1):
            xt, hT = self.load_hT(l, m, src, n)
            for j, col in enumerate((O_CC, O_CC + 128, O_CX, O_CX + 128)):
                self.proj_fm(hT, ps[2], j, col)
            self.proj_tm(hT, ps[3], O_K, 512)
            self.proj_tm(hT, ps[4], O_V, 512)
            cxs = wk["cxs"]
            self.cp("act", cxs[:, :, :], ps[2][:, 256:512].rearrange("p (c t) -> p c t", t=128), [ps[2]], [cxs])
            self.tt("dve", zall[:, :, ZPAD + n * 128:ZPAD + (n + 1) * 128], ps[2][:, 0:256].rearrange("p (c t) -> p c t", t=128),
                    cxs[:, :, :], ALU.mult, [ps[2], cxs], [zall])
            ktok, vd = wk["ktok"], wk["vd"]
            self.cp("act", ktok[:, :], ps[3][:, :], [ps[3]], [ktok])
            self.tt("dve", vd[:, :].rearrange("p (h e) -> p h e", e=64), ps[4][:, :].rearrange("p (h e) -> p h e", e=64),
                    kd[:, 1, :].unsqueeze(2).to_broadcast([128, 8, 64]), ALU.mult, [ps[4], kd], [vd])
            dst = (sball[:, n - 1, :, :], sball) if n >= 1 else None
            self.state_update(1, ps[5], ktok, vd, dst)
            if fwd_final:
                vdf = wk["vbf"]
                self.tt("dve", vdf[:, :].rearrange("p (h e) -> p h e", e=64), ps[4][:, :].rearrange("p (h e) -> p h e", e=64),
                        kd[:, 0, :].unsqueeze(2).to_broadcast([128, 8, 64]), ALU.mult, [ps[4], kd], [vdf])
                assert N == 2
                self.state_update(0, ps[6], ktok, vdf, None, fwd_weight=("one" if n == N - 1 else "cd"))

    def mainpass(self, l, m, src, dst, N, grid):
        S, ps, wk, W = self.S, self.ps, self.wk, self.W
        win, wout = W["win"], W["wout"]
        zall, sball, kd, tq, dcomb = W["zall"], W["sball"], W["kd"], W["tq"], W["dcomb"]
        st32, sfbf = W["st32"], W["sfbf"]
        self.cp("act", sfbf[:, :, :], st32[0][:, :, :], [st32[0]], [sfbf])
        for n in range(N):
            xt, hT = self.load_hT(l, m, src, n)
            for j, col in enumerate((O_CB, O_CB + 128, O_U, O_U + 128)):
                self.proj_fm(hT, ps[2], j, col)
            for c in range(4):
                self.proj_fm(hT, ps[3], c, O_Q + c * 128)
            for c in range(4):
                self.proj_fm(hT, ps[4], c, O_K + c * 128)
            self.proj_tm(hT, ps[5], O_V, 512)
            self.proj_tm(hT, ps[6], O_G, 512)
            self.proj_tm(hT, ps[7], O_K, 512)
            self.proj_tm(hT, ps[0], O_VV, 256)
            if CUT == 1:
                return
            q8T, qm, kT = wk["q8T"], wk["qm"], wk["kT"]
            tqm = W["tqm"]
            cbs, uT = wk["cbs"], wk["uT"]
            self.cp("act", q8T[:, :, :], ps[3][:, :].rearrange("p (c t) -> p c t", t=128), [ps[3]], [q8T])
            self.cp("act", kT[:, :, :], ps[4][:, :].rearrange("p (c t) -> p c t", t=128), [ps[4]], [kT])
            self.cp("act", cbs[:, :, :], ps[2][:, 0:256].rearrange("p (c t) -> p c t", t=128), [ps[2]], [cbs])
            self.cp("act", uT[:, :, :], ps[2][:, 256:512].rearrange("p (c t) -> p c t", t=128), [ps[2]], [uT])
            for vh in range(6):
                self.tt("dve", qm[:, vh, :, :], q8T[:, :, :], tqm[:, vh // 2, vh % 2, :, :], ALU.mult, [q8T, tqm], [qm])
            ktok, vbf, vd, sg = wk["ktok"], wk["vbf"], wk["vd"], wk["sg"]
            self.cp("act", vbf[:, :], ps[5][:, :], [ps[5]], [vbf])
            self.tt("dve", vd[:, :].rearrange("p (h e) -> p h e", e=64), ps[5][:, :].rearrange("p (h e) -> p h e", e=64),
                    kd[:, 0, :].unsqueeze(2).to_broadcast([128, 8, 64]), ALU.mult, [ps[5], kd], [vd])
            self.cp("act", ktok[:, :], ps[7][:, :], [ps[7]], [ktok])
            if SUB == 3:
                return
            self.sigmoid(sg[:, :], ps[6][:, :], [ps[6]], [sg])
            self.tt("dve", sg[:, :], sg[:, :], ps[6][:, :], ALU.mult, [sg, ps[6]], [sg])
            if CUT == 2:
                return
            vst, vn = wk["vst"], wk["vn"]
            S.op("dve", lambda e: e.bn_stats(vst[:, 0:6], ps[0][:, 0:256]), [ps[0]], [vst])
            S.op("dve", lambda e: e.bn_aggr(vst[:, 6:8], vst[:, 0:6]), [vst], [vst])
            self.act(vst[:, 8:9], vst[:, 7:8], AF.Ln, [vst, self.epsT], [vst], bias=self.epsT[:, 0:1], scale=1.0)
            self.act(vst[:, 9:10], vst[:, 8:9], AF.Exp, [vst], [vst], scale=-0.5)
            self.ts("dve", vn[:, :], ps[0][:, 0:256], vst[:, 6:7], vst[:, 9:10], ALU.subtract, ALU.mult, [ps[0], vst], [vn])
            if CUT == 3:
                return
            msk = wk["msk"]
            for h in range(8):
                c, hp = h // 2, h % 2
                bank = ps[h // 4]
                self.mm(bank[:, (h % 4) * 128:(h % 4 + 1) * 128], kT[:, c, :], qm[:, 0 + hp, c, :],
                        True, True, [kT, qm], [bank])
            for b2 in range(2):
                self.tt("dve", msk[:, b2 * 4:(b2 + 1) * 4, :], ps[b2][:, :].rearrange("p (h t) -> p h t", t=128),
                        dcomb[:, b2 * 4:(b2 + 1) * 4, :], ALU.mult, [ps[b2], dcomb], [msk])
            if CUT == 4:
                return
            for h in range(8):
                c, hp = h // 2, h % 2
                p0, p1 = hp * 64, hp * 64 + 64
                o_ap = ps[3][:, h * 64:(h + 1) * 64]
                self.mm(o_ap, msk[:, h, :], vbf[:, h * 64:(h + 1) * 64], True, False, [msk, vbf], [ps[3]])
                self.mm(o_ap, qm[:, 2 + hp, c, :], sfbf[:, c, :], False, False, [qm, sfbf], [ps[3]])
                self.mm(o_ap, qm[:, 4 + hp, c, :], sball[:, n, c, :], False, True, [qm, sball], [ps[3]])
            if CUT == 5:
                return
            self.state_update(0, ps[5], ktok, vd, (sfbf[:, :, :], sfbf))
            if CUT == 6:
                return
            osb, osq, gst, yb = wk["osb"], wk["osq"], wk["gst"], wk["yb"]
            self.cp("act", osb[:, :], ps[3][:, :], [ps[3]], [osb])
            self.tt("dve", osq[:, :], osb[:, :], osb[:, :], ALU.mult, [osb], [osq])
            self.red(gst[:, 0, :], osb[:, :].rearrange("p (h e) -> p h e", e=64), ALU.add, [osb], [gst])
            self.red(gst[:, 1, :], osq[:, :].rearrange("p (h e) -> p h e", e=64), ALU.add, [osq], [gst])
            self.ts("dve", gst[:, 0:2, :], gst[:, 0:2, :], 1.0 / 64, None, ALU.mult, None, [gst], [gst])
            self.tt("dve", gst[:, 2, :], gst[:, 0, :], gst[:, 0, :], ALU.mult, [gst], [gst])
            self.tt("dve", gst[:, 1, :], gst[:, 1, :], gst[:, 2, :], ALU.subtract, [gst], [gst])
            self.act(gst[:, 2, :], gst[:, 1, :], AF.Ln, [gst, self.epsT], [gst], bias=self.epsT[:, 0:1], scale=1.0)
            self.act(gst[:, 3, :], gst[:, 2, :], AF.Exp, [gst], [gst], scale=-0.5)
            o3 = osb[:, :].rearrange("p (h e) -> p h e", e=64)
            self.tt("dve", o3, o3, gst[:, 0, :].unsqueeze(2).to_broadcast([128, 8, 64]), ALU.subtract, [osb, gst], [osb])
            self.tt("dve", o3, o3, gst[:, 3, :].unsqueeze(2).to_broadcast([128, 8, 64]), ALU.mult, [osb, gst], [osb])
            self.tt("dve", yb[:, :], osb[:, :], sg[:, :], ALU.mult, [osb, sg], [yb])
            if CUT == 7:
                return
            mixT = wk["mixT"]
            for c in range(4):
                self.tr(ps[4][:, c * 128:(c + 1) * 128], yb[:, c * 128:(c + 1) * 128], [yb], [ps[4]])
            self.cp("act", mixT[:, 2:6, :], ps[4][:, :].rearrange("p (c t) -> p c t", t=128), [ps[4]], [mixT])
            if CUT == 8:
                return
            wsT, bsT = W["wsT"], W["bsT"]
            for g in range(4):
                gp, gi = g // 2, g % 2
                self.mm(ps[7][:, g * 128:(g + 1) * 128], vn[:, gp * 128:(gp + 1) * 128], wsT[:, g, :], True, True,
                        [vn, wsT], [ps[7]])
            mx = wk["mx"]
            for g in range(4):
                gp, gi = g // 2, g % 2
                self.tt("dve", mx[gi * 64:(gi + 1) * 64, gp, :], ps[7][gi * 64:(gi + 1) * 64, g * 128:(g + 1) * 128],
                        bsT[gi * 64:(gi + 1) * 64, gp, :], ALU.add, [ps[7], bsT], [mx])
            self.tt("dve", mixT[:, 6:8, :], mx[:, :, :], uT[:, :, :], ALU.mult, [mx, uT], [mixT])
            if CUT == 9:
                return
            cv, cw = wk["cv"], W["cw"]
            t0 = ZPAD + n * 128
            if grid:
                z0 = zall[:, 0, t0:t0 + 128].rearrange("p (r w) -> p r w", w=64)
                c0 = cv[:, 0, :].rearrange("p (r w) -> p r w", w=64)
                self.ts("dve", cv[:, 0, :], zall[:, 0, t0:t0 + 128], cw[:, 0, 1:2], None, ALU.mult, None, [zall, cw], [cv])
                self.stt(c0[:, :, 1:64], z0[:, :, 0:63], cw[:, 0, 0:1], c0[:, :, 1:64], ALU.mult, ALU.add, [zall, cw, cv], [cv])
                self.stt(c0[:, :, 0:63], z0[:, :, 1:64], cw[:, 0, 2:3], c0[:, :, 0:63], ALU.mult, ALU.add, [zall, cw, cv], [cv])
                self.ts("dve", cv[:, 1, :], zall[:, 1, t0:t0 + 128], cw[:, 1, 1:2], None, ALU.mult, None, [zall, cw], [cv])
                self.stt(cv[:, 1, :], zall[:, 1, t0 - 64:t0 + 64], cw[:, 1, 0:1], cv[:, 1, :], ALU.mult, ALU.add, [zall, cw, cv], [cv])
                self.stt(cv[:, 1, :], zall[:, 1, t0 + 64:t0 + 192], cw[:, 1, 2:3], cv[:, 1, :], ALU.mult, ALU.add, [zall, cw, cv], [cv])
            else:
                for ch in range(2):
                    self.ts("dve", cv[:, ch, :], zall[:, ch, t0:t0 + 128], cw[:, ch, 1:2], None, ALU.mult, None, [zall, cw], [cv])
                    self.stt(cv[:, ch, :], zall[:, ch, t0 - 1:t0 + 127], cw[:, ch, 0:1], cv[:, ch, :], ALU.mult, ALU.add, [zall, cw, cv], [cv])
                    self.stt(cv[:, ch, :], zall[:, ch, t0 + 1:t0 + 129], cw[:, ch, 2:3], cv[:, ch, :], ALU.mult, ALU.add, [zall, cw, cv], [cv])
            self.tt("dve", mixT[:, 0:2, :], cv[:, :, :], cbs[:, :, :], ALU.mult, [cv, cbs], [mixT])
            if CUT == 10:
                return
            for hh, bank in ((0, ps[6]), (1, ps[1])):
                for kc in range(8):
                    self.mm(bank[:, :], mixT[:, kc, :], wout[:, kc, hh * 512:(hh + 1) * 512], kc == 0, kc == 7, [mixT, wout], [bank])
            if CUT == 11:
                return
            r, g1 = wk["r"], W["g1"]
            for hh, bank in ((0, ps[6]), (1, ps[1])):
                self.tt("dve", r[:, hh * 512:(hh + 1) * 512], bank[:, :], g1[:, hh * 512:(hh + 1) * 512], ALU.mult, [bank, g1], [r])
            self.stt(r[:, :], xt[:, :], ALPHA, r[:, :], ALU.mult, ALU.add, [xt, r], [r])
            xo = wk["xo"][n % 2]
            self.layernorm(r, W["lng"], W["lnb"], xo, wk["lnsc"])
            self.dma("sp", dst[n][:, :], xo[:, :], [xo], [dst[n]], xo)

    def phase_moe(self, l):
        S, I, ps, nc = self.S, self.I, self.ps, self.nc
        last = l == DEPTH - 1
        tiles = []
        if not last:
            for n in range(NCC):
                tiles.append((1, self.c1[l][n], self.c2[l][n]))
        for n in range(NCH):
            tiles.append((0, self.x1[l][n], self.outt[n] if last else self.x2[l][n]))
        ngrp = 3
        per = -(-len(tiles) // ngrp)
        groups = [tiles[i * per:(i + 1) * per] for i in range(ngrp)]
        GMAX = per
        with contextlib.ExitStack() as st:
            sb = lambda name, shape, dt=F32: self.sb(st, name, shape, dt)
            g2 = [sb("g2_%d" % m, [128, D]) for m in range(2)]
            lng = sb("lng2", [128, D])
            lnb = sb("lnb2", [128, D])
            for m in range(2):
                self.dma("sp", g2[m][:, :], self.modd.t[l, m, 0, 5120:6144].partition_broadcast(128), [self.modd], [g2[m]], g2[m])
            self.dma("sp", lng[:, :], I["ln2_g"].t[l].partition_broadcast(128), [I["ln2_g"]], [lng], lng)
            self.dma("sp", lnb[:, :], I["ln2_b"].t[l].partition_broadcast(128), [I["ln2_b"]], [lnb], lnb)
            modT = sb("modT2", [128, 2, 2, 8])
            for m in range(2):
                self.dma("sp", modT[:, m, 0, :], self.modd.t[l, m, 0, 3072:4096].rearrange("(k p) -> p k", p=128),
                         [self.modd], [modT], modT, allow_slow_non_contiguous=True)
                self.dma("sp", modT[:, m, 1, :], self.modd.t[l, m, 1, 4096:5120].rearrange("(k p) -> p k", p=128),
                         [self.modd], [modT], modT, allow_slow_non_contiguous=True)
            wr = sb("wr", [128, 8, 36])
            brt = sb("brt", [128, 36])
            self.dma("sp", wr[:, :, :], I["wr"].t[l].rearrange("(k p) c -> p k c", p=128), [I["wr"]], [wr], wr)
            self.dma("sp", brt[:, :], I["br"].t[l].partition_broadcast(128), [I["br"]], [brt], brt)
            yacc = [sb("yacc%d" % i, [128, D]) for i in range(GMAX)]
            hmT = [sb("hmT%d" % i, [128, 8, 512], BF16) for i in range((GMAX + 3) // 4)]
            gate = [sb("gate%d" % i, [128, 32]) for i in range(GMAX)]
            hmf = sb("hmf", [128, 8, 128])
            wgs = [sb("wgs%d" % i, [128, 8, EH], BF16) for i in range(2)]
            wus = [sb("wus%d" % i, [128, 8, EH], BF16) for i in range(2)]
            wds = [sb("wds%d" % i, [128, 4, D], BF16) for i in range(2)]
            xt = [sb("mxt%d" % i, [128, D]) for i in range(2)]
            xo = [sb("mxo%d" % i, [128, D]) for i in range(2)]
            r = sb("mr", [128, D])
            sgs = [sb("sgs%d" % i, [128, 512]) for i in range(2)]
            hT = [sb("hhT%d" % i, [128, 4, 512], BF16) for i in range(2)]
            rt = sb("rt", [128, 128])
            lnsc = dict(st=sb("lnst2", [128, 12]), mv=sb("lnmv2", [128, 2]), rs=sb("lnrs2", [128, 2]))
            wcount = 0
            hcount = 0
            for grp in groups:
                G = len(grp)
                for ti, (m, src, dst) in enumerate(grp):
                    x_t = xt[ti % 2]
                    self.dma("sp", x_t[:, :], src[:, :], [src], [x_t], x_t)
                    for kc in range(8):
                        self.tr(ps[kc // 4][:, (kc % 4) * 128:(kc % 4 + 1) * 128], x_t[:, kc * 128:(kc + 1) * 128], [x_t], [ps[kc // 4]])
                    for kc in range(8):
                        src_ps = ps[kc // 4][:, (kc % 4) * 128:(kc % 4 + 1) * 128]
                        self.act(hmf[:, kc, :], src_ps, AF.Identity, [ps[kc // 4], modT], [hmf],
                                 bias=modT[:, m, 0, kc:kc + 1], scale=modT[:, m, 1, kc:kc + 1])
                    self.cp("dve", hmT[ti // 4][:, :, (ti % 4) * 128:(ti % 4 + 1) * 128], hmf[:, :, :], [hmf], [hmT[ti // 4]])
                    for kc in range(8):
                        self.mm(ps[2][:, 0:36], hmf[:, kc, :], wr[:, kc, :], kc == 0, kc == 7, [hmf, wr], [ps[2]])
                    self.tt("dve", rt[:, 0:36], ps[2][:, 0:36], brt[:, :], ALU.add, [ps[2], brt], [rt])
                    self.red(rt[:, 40:41], rt[:, 0:4], ALU.max, [rt], [rt])
                    self.ts("dve", rt[:, 41:42], rt[:, 40:41], -1.0, None, ALU.mult, None, [rt], [rt])
                    self.act(rt[:, 44:48], rt[:, 0:4], AF.Exp, [rt], [rt], bias=rt[:, 41:42], scale=1.0)
                    self.red(rt[:, 42:43], rt[:, 44:48], ALU.add, [rt], [rt])
                    S.op("dve", lambda e: e.reciprocal(rt[:, 43:44], rt[:, 42:43]), [rt], [rt])
                    self.ts("dve", rt[:, 48:52], rt[:, 0:4], rt[:, 40:41], None, ALU.is_equal, None, [rt], [rt])
                    self.tt("dve", rt[:, 64:96].rearrange("p (g e) -> p g e", e=8), rt[:, 4:36].rearrange("p (g e) -> p g e", e=8),
                            rt[:, 48:52].unsqueeze(2).to_broadcast([128, 4, 8]), ALU.mult, [rt], [rt])
                    self.red(rt[:, 96:104], rt[:, 64:96].rearrange("p (g e) -> p e g", e=8), ALU.add, [rt], [rt])
                    S.op("dve", lambda e: e.max(rt[:, 104:112], rt[:, 96:104]), [rt], [rt])
                    self.ts("dve", rt[:, 112:120], rt[:, 96:104], rt[:, 105:106], None, ALU.is_ge, None, [rt], [rt])
                    self.ts("dve", rt[:, 52:53], rt[:, 104:105], -1.0, None, ALU.mult, None, [rt], [rt])
                    self.act(rt[:, 120:128], rt[:, 96:104], AF.Exp, [rt], [rt], bias=rt[:, 52:53], scale=1.0)
                    self.tt("dve", rt[:, 120:128], rt[:, 120:128], rt[:, 112:120], ALU.mult, [rt], [rt])
                    self.red(rt[:, 53:54], rt[:, 120:128], ALU.add, [rt], [rt])
                    S.op("dve", lambda e: e.reciprocal(rt[:, 54:55], rt[:, 53:54]), [rt], [rt])
                    self.tt("dve", rt[:, 54:55], rt[:, 54:55], rt[:, 43:44], ALU.mult, [rt], [rt])
                    self.ts("dve", rt[:, 120:128], rt[:, 120:128], rt[:, 54:55], None, ALU.mult, None, [rt], [rt])
                    self.tt("dve", gate[ti][:, :].rearrange("p (g e) -> p g e", e=8),
                            rt[:, 48:52].unsqueeze(2).to_broadcast([128, 4, 8]),
                            rt[:, 120:128].unsqueeze(1).to_broadcast([128, 4, 8]), ALU.mult, [rt], [gate[ti]])
                mts = [list(range(i, min(i + 4, G))) for i in range(0, G, 4)]
                for e_ in range(NE):
                    wg, wu, wd = wgs[wcount % 2], wus[wcount % 2], wds[wcount % 2]
                    wcount += 1
                    for k4 in range(2):
                        self.dma("pool", wg[:, k4 * 4:(k4 + 1) * 4, :],
                                 I["wg"].t[l, e_, k4 * 512:(k4 + 1) * 512, :].rearrange("(k p) c -> p k c", p=128),
                                 [I["wg"]], [wg], wg)
                        self.dma("pool", wu[:, k4 * 4:(k4 + 1) * 4, :],
                                 I["wu"].t[l, e_, k4 * 512:(k4 + 1) * 512, :].rearrange("(k p) c -> p k c", p=128),
                                 [I["wu"]], [wu], wu)
                    self.dma("pool", wd[:, :, :], I["wd"].t[l, e_].rearrange("(k p) c -> p k c", p=128), [I["wd"]], [wd], wd)
                    for mt in mts:
                        nt = len(mt)
                        hTt = hT[hcount % 2]
                        hcount += 1
                        for hc in range(4):
                            pg, pu = ps[hc % 2], ps[2 + hc % 2]
                            for (bank, w_) in ((pg, wg), (pu, wu)):
                                for kc in range(8):
                                    self.mm(bank[:, 0:nt * 128], w_[:, kc, hc * 128:(hc + 1) * 128], hmT[mt[0] // 4][:, kc, 0:nt * 128],
                                            kc == 0, kc == 7, [w_, hmT[mt[0] // 4]], [bank])
                            sg_ = sgs[hc % 2]
                            self.sigmoid(sg_[:, 0:nt * 128], pg[:, 0:nt * 128], [pg], [sg_])
                            self.tt("dve", sg_[:, 0:nt * 128], sg_[:, 0:nt * 128], pg[:, 0:nt * 128], ALU.mult, [sg_, pg], [sg_])
                            self.tt("dve", hTt[:, hc, 0:nt * 128], sg_[:, 0:nt * 128], pu[:, 0:nt * 128], ALU.mult, [sg_, pu], [hTt])
                        for j, ti in enumerate(mt):
                            for hh in range(2):
                                bank = ps[4 + 2 * (j % 2) + hh]
                                for hc in range(4):
                                    self.mm(bank[:, :], hTt[:, hc, j * 128:(j + 1) * 128], wd[:, hc, hh * 512:(hh + 1) * 512], hc == 0, hc == 3,
                                            [hTt, wd], [bank])
                                ya = yacc[ti][:, hh * 512:(hh + 1) * 512]
                                if e_ == 0:
                                    self.ts("dve", ya, bank[:, :], gate[ti][:, 0:1], None, ALU.mult, None, [bank, gate[ti]], [yacc[ti]])
                                else:
                                    self.stt(ya, bank[:, :], gate[ti][:, e_:e_ + 1], ya, ALU.mult, ALU.add, [bank, gate[ti], yacc[ti]], [yacc[ti]])
                for ti, (m, src, dst) in enumerate(grp):
                    x_t = xt[ti % 2]
                    self.dma("sp", x_t[:, :], src[:, :], [src], [x_t], x_t)
                    self.tt("pool", r[:, :], yacc[ti][:, :], g2[m][:, :], ALU.mult, [yacc[ti], g2[m]], [r])
                    self.stt(r[:, :], x_t[:, :], ALPHA, r[:, :], ALU.mult, ALU.add, [x_t, r], [r])
                    xo_ = xo[ti % 2]
                    self.layernorm(r, lng, lnb, xo_, lnsc)
                    self.dma("sp", dst[:, :], xo_[:, :], [xo_], [dst], xo_)
            S.flush()


def _consts():
    c = np.zeros((128, 8, 128), np.float32)
    j = np.arange(128)[:, None].astype(np.float32)
    i = np.arange(128)[None, :].astype(np.float32)
    c[:, 0, :] = np.eye(128, dtype=np.float32)
    c[:, 1, :] = np.maximum(i - j, 0)
    c[:, 2, :] = (i >= j)
    c[:, 3, :] = np.maximum(j - i, 0)
    c[:, 4, :] = (j >= i)
    c[:, 5, :] = i + 1.0
    c[:, 6, :] = 128.0 - i
    c[:, 7, 0] = 127.0 - j[:, 0]
    c[:, 7, 1] = j[:, 0]
    c[:64, 7, 2] = 1.0
    c[64:, 7, 3] = 1.0
    return c.reshape(128, 8 * 128)


def make_in_maps(inputs):
    f = lambda a: np.ascontiguousarray(np.asarray(a, dtype=np.float32))
    wr = np.concatenate([f(inputs["router_group_w"])] + [f(inputs["router_expert_w"])[:, g] for g in range(4)], axis=2)
    br = np.concatenate([f(inputs["router_group_b"]), f(inputs["router_expert_b"]).reshape(DEPTH, 32)], axis=1)
    shared = {
        "w_ada": f(inputs["w_ada"]), "b_ada": f(inputs["b_ada"]), "w_in": f(inputs["w_in"]), "conv_w": f(inputs["conv_w"]),
        "dec_f": f(inputs["ret_decay_fwd"]), "dec_b": f(inputs["ret_decay_bwd"]), "sgu_w": f(inputs["sgu_w"]),
        "sgu_b": f(inputs["sgu_b"]), "w_out": f(inputs["w_out"]), "ln1_g": f(inputs["ln1_g"]), "ln1_b": f(inputs["ln1_b"]),
        "wr": f(wr), "br": f(br), "wg": f(inputs["moe_w_gate"]), "wu": f(inputs["moe_w_up"]), "wd": f(inputs["moe_w_down"]),
        "ln2_g": f(inputs["ln2_g"]), "ln2_b": f(inputs["ln2_b"]), "consts": _consts(),
    }
    x, c, ctx, c_ctx = f(inputs["x"]), f(inputs["c"]), f(inputs["ctx"]), f(inputs["c_ctx"])
    maps = []
    for b in range(8):
        cT = np.stack([c[b].reshape(8, 128).T, c_ctx.reshape(8, 128).T], axis=2).reshape(128, 16)
        d = dict(shared)
        d.update({"x": x[b], "ctx": ctx[b], "cT": f(cT)})
        maps.append(d)
    return maps


def kernel(**inputs):
    nc = KB().build()
    maps = make_in_maps(inputs)
    res = run_bass_kernel_spmd(nc, maps, core_ids=list(range(8)))
    return np.stack([np.asarray(r["out"]) for r in res.results], axis=0).astype(np.float32)
```

```python
import contextlib
import numpy as np
import concourse.bass as bass
import concourse.mybir as mybir
from concourse.bass_utils import run_bass_kernel_spmd

F32 = mybir.dt.float32
BF16 = mybir.dt.bfloat16
AF = mybir.ActivationFunctionType
ALU = mybir.AluOpType
AX = mybir.AxisListType

D = 1024
SEQ = 4096
CTX = 256
DEPTH = 2
NCH = SEQ // 128
NCC = CTX // 128
IN_COLS = 3328
O_CB, O_CC, O_CX, O_Q, O_K, O_V, O_G, O_U, O_VV = 0, 256, 512, 768, 1280, 1792, 2304, 2816, 3072
NE = 32
EH = 512
ALPHA = float((2 * DEPTH) ** 0.25)
EPS = 1e-5
ZPAD = 64
import os
CUT = int(os.environ.get('MP_CUT', '0'))
SUB = int(os.environ.get('MP_SUB', '0'))
SIG = int(os.environ.get('MP_SIG', '1'))


class Buf:
    __slots__ = ("name", "w", "r", "psum")

    def __init__(self, name):
        self.name = name
        self.w = None
        self.r = []
        self.psum = False


class Tl:
    def __init__(self, t, name):
        self.t = t
        self.b = Buf(name)

    def __getitem__(self, k):
        return self.t[k]


class Sched:
    CE = ("pe", "dve", "act", "pool")

    def __init__(self, nc, stack):
        self.nc = nc
        self.stack = stack
        self.eng = {"pe": nc.tensor, "dve": nc.vector, "act": nc.scalar, "pool": nc.gpsimd, "sp": nc.sync}
        self.sem = {e: stack.enter_context(nc.semaphore("s_" + e)) for e in self.CE}
        self.cnt = {e: 0 for e in self.CE}
        self.known = {e: {} for e in self.eng}
        self.ops = {e: [] for e in self.eng}
        self.dsem = {}
        self.dcnt = {}
        self.semname = {}
        self.nops = 0

    def _dma_sem(self, buf):
        k = id(buf)
        if k not in self.dsem:
            s = self.stack.enter_context(self.nc.semaphore("d%d" % len(self.dsem)))
            self.dsem[k] = s
            self.dcnt[k] = 0
        return k

    def op(self, e, fn, R=(), W=(), dma=None):
        deps = []
        for t in R:
            if t.b.w is not None:
                deps.append(t.b.w)
            if t.b.psum:
                deps.extend(r_ for r_ in t.b.r if r_[0] != ("c", e))
        dk = ("d", self._dma_sem(dma.b)) if dma is not None else None
        for t in W:
            if t.b.w is not None and not (dk is not None and t.b.w[0] == dk):
                deps.append(t.b.w)
            deps.extend(t.b.r)
        if dma is not None:
            k = dk[1]
            self.dcnt[k] += 16
            tok = (("d", k), self.dcnt[k])
            inc = (self.dsem[k], 16)
        else:
            self.cnt[e] += 1
            tok = (("c", e), self.cnt[e])
            inc = (self.sem[e], 1)
        waits = []
        kn = self.known[e]
        for (sk, v) in deps:
            if sk == ("c", "pe") and e == "pe" and dma is None:
                continue
            if kn.get(sk, 0) >= v:
                continue
            kn[sk] = v
            waits.append((self.sem[sk[1]] if sk[0] == "c" else self.dsem[sk[1]], v))
        self.ops[e].append((waits, fn, inc))
        for t in R:
            t.b.r.append(tok)
        for t in W:
            t.b.w = tok
            t.b.r = []
        self.nops += 1

    def drain(self):
        waits = []
        kn = self.known["sp"]
        for k, c in self.dcnt.items():
            if c and kn.get(("d", k), 0) < c:
                kn[("d", k)] = c
                waits.append((self.dsem[k], c))
        for e in self.CE:
            if self.cnt[e] and kn.get(("c", e), 0) < self.cnt[e]:
                kn[("c", e)] = self.cnt[e]
                waits.append((self.sem[e], self.cnt[e]))
        self.ops["sp"].append((waits, None, None))

    def flush(self):
        self.drain()
        ops = self.ops
        with self.nc.Block() as block:
            def emit(eng, lst):
                for waits, fn, inc in lst:
                    for s, v in waits:
                        eng.wait_ge(s, v)
                    if fn is not None:
                        fn(eng).then_inc(inc[0], inc[1])

            if ops["pe"]:
                @block.tensor
                def _(eng):
                    emit(eng, ops["pe"])
            if ops["dve"]:
                @block.vector
                def _(eng):
                    emit(eng, ops["dve"])
            if ops["act"]:
                @block.scalar
                def _(eng):
                    emit(eng, ops["act"])
            if ops["pool"]:
                @block.gpsimd
                def _(eng):
                    emit(eng, ops["pool"])
            if ops["sp"]:
                @block.sync
                def _(eng):
                    emit(eng, ops["sp"])
        self.ops = {e: [] for e in self.eng}


class KB:
    def __init__(self, debug=False, stop=None):
        self.debug = debug
        self.stop = stop
        self.nc = bass.Bass("TRN2", target_bir_lowering=False)
        self.root = contextlib.ExitStack()

    def dram_in(self, name, shape):
        return Tl(self.nc.dram_tensor(name, list(shape), F32, kind="ExternalInput").ap(), name)

    def dram_tmp(self, name, shape, out=False):
        kind = "ExternalOutput" if (out or self.debug) else "Internal"
        return Tl(self.nc.dram_tensor(name, list(shape), F32, kind=kind).ap(), name)

    def sb(self, stack, name, shape, dt=F32):
        self.uid = getattr(self, "uid", 0) + 1
        name = "sb%d_%s" % (self.uid, name)
        return Tl(stack.enter_context(self.nc.sbuf_tensor(name, list(shape), dt)), name)

    def mm(self, out, lhsT, rhs, start, stop, R, W):
        self.S.op("pe", lambda e: e.matmul(out, lhsT, rhs, start=start, stop=stop), R, W)

    def tr(self, out, in_, R, W):
        ident = self.ident[:, :]
        self.S.op("pe", lambda e: e.transpose(out, in_, ident), list(R) + [self.ident], W)

    def dma(self, q, out, in_, R, W, sem, **kw):
        self.S.op(q, lambda e: e.dma_start(out, in_, **kw), R, W, dma=sem)

    def act(self, out, in_, func, R, W, bias=None, scale=None, accum=None, eng="act"):
        kw = {}
        if bias is not None:
            kw["bias"] = bias
        if scale is not None:
            kw["scale"] = scale
        if accum is not None:
            kw["accum_out"] = accum
        self.S.op(eng, lambda e: e.activation(out, in_, func, **kw), R, W)

    def sigmoid(self, out_ap, in_ap, R, W):
        if SIG:
            self.act(out_ap, in_ap, AF.Sigmoid, R, W)
            return
        self.act(out_ap, in_ap, AF.Exp, R, W, scale=-1.0)
        self.act(out_ap, out_ap, AF.Ln, W, W, bias=1.0, scale=1.0)
        self.act(out_ap, out_ap, AF.Exp, W, W, scale=-1.0)

    def tt(self, eng, out, a, b, op, R, W):
        self.S.op(eng, lambda e: e.tensor_tensor(out, a, b, op), R, W)

    def ts(self, eng, out, a, s1, s2, op0, op1, R, W):
        if op1 is None:
            self.S.op(eng, lambda e: e.tensor_scalar(out, a, s1, None, op0), R, W)
        else:
            self.S.op(eng, lambda e: e.tensor_scalar(out, a, s1, s2, op0, op1), R, W)

    def stt(self, out, a, s, b, op0, op1, R, W):
        self.S.op("dve", lambda e: e.scalar_tensor_tensor(out, a, s, b, op0, op1), R, W)

    def cp(self, eng, out, in_, R, W):
        if eng == "act":
            self.S.op("act", lambda e: e.activation(out, in_, AF.Copy), R, W)
        else:
            self.S.op(eng, lambda e: e.tensor_copy(out, in_), R, W)

    def red(self, out, in_, op, R, W, axis=AX.X):
        self.S.op("dve", lambda e: e.tensor_reduce(out, in_, axis, op), R, W)

    def layernorm(self, r, gam, bet, out, sc):
        st, mv, rs = sc["st"], sc["mv"], sc["rs"]
        for hh in range(2):
            self.S.op("dve", lambda e, hh=hh: e.bn_stats(st[:, hh * 6:(hh + 1) * 6], r[:, hh * 512:(hh + 1) * 512]), [r], [st])
        self.S.op("dve", lambda e: e.bn_aggr(mv[:, 0:2], st[:, 0:12]), [st], [mv])
        self.act(rs[:, 0:1], mv[:, 1:2], AF.Ln, [mv, self.epsT], [rs], bias=self.epsT[:, 0:1], scale=1.0)
        self.act(rs[:, 1:2], rs[:, 0:1], AF.Exp, [rs], [rs], scale=-0.5)
        self.ts("dve", r[:, :], r[:, :], mv[:, 0:1], rs[:, 1:2], ALU.subtract, ALU.mult, [r, mv, rs], [r])
        self.tt("pool", r[:, :], r[:, :], gam[:, :], ALU.mult, [r, gam], [r])
        self.tt("dve", out[:, :], r[:, :], bet[:, :], ALU.add, [r, bet], [out])

    def build(self):
        nc = self.nc
        root = self.root
        S = self.S = Sched(nc, root)
        I = {}
        I["x"] = self.dram_in("x", [SEQ, D])
        I["ctx"] = self.dram_in("ctx", [CTX, D])
        I["cT"] = self.dram_in("cT", [128, 16])
        I["w_ada"] = self.dram_in("w_ada", [DEPTH, D, 6 * D])
        I["b_ada"] = self.dram_in("b_ada", [DEPTH, 6 * D])
        I["w_in"] = self.dram_in("w_in", [DEPTH, D, IN_COLS])
        I["conv_w"] = self.dram_in("conv_w", [DEPTH, 3, 256])
        I["dec_f"] = self.dram_in("dec_f", [DEPTH, 8])
        I["dec_b"] = self.dram_in("dec_b", [DEPTH, 8])
        I["sgu_w"] = self.dram_in("sgu_w", [DEPTH, 4, 128, 128])
        I["sgu_b"] = self.dram_in("sgu_b", [DEPTH, 4, 128])
        I["w_out"] = self.dram_in("w_out", [DEPTH, D, D])
        I["ln1_g"] = self.dram_in("ln1_g", [DEPTH, D])
        I["ln1_b"] = self.dram_in("ln1_b", [DEPTH, D])
        I["wr"] = self.dram_in("wr", [DEPTH, D, 36])
        I["br"] = self.dram_in("br", [DEPTH, 36])
        nes = 1 if (self.stop or "").startswith("mi") or self.stop == "mod" else NE
        I["wg"] = self.dram_in("wg", [DEPTH, nes, D, EH])
        I["wu"] = self.dram_in("wu", [DEPTH, nes, D, EH])
        I["wd"] = self.dram_in("wd", [DEPTH, nes, EH, D])
        I["ln2_g"] = self.dram_in("ln2_g", [DEPTH, D])
        I["ln2_b"] = self.dram_in("ln2_b", [DEPTH, D])
        I["consts"] = self.dram_in("consts", [128, 8 * 128])
        self.I = I
        out = self.out = self.dram_tmp("out", [SEQ, D], out=True)
        modd = self.modd = self.dram_tmp("modd", [DEPTH, 2, 2, 6 * D])
        def scr(name, n):
            t = self.dram_tmp(name, [n * 128, D])
            return [Tl(t.t[i * 128:(i + 1) * 128, :], "%s_%d" % (name, i)) for i in range(n)]
        self.x1 = [scr("x1_%d" % l, NCH) for l in range(DEPTH)]
        self.x2 = [scr("x2_%d" % l, NCH) for l in range(DEPTH - 1)]
        self.c1 = [scr("c1_%d" % l, NCC) for l in range(DEPTH - 1)]
        self.c2 = [scr("c2_%d" % l, NCC) for l in range(DEPTH - 1)]
        self.xin = [Tl(I["x"].t[i * 128:(i + 1) * 128, :], "xin%d" % i) for i in range(NCH)]
        self.cin = [Tl(I["ctx"].t[i * 128:(i + 1) * 128, :], "cin%d" % i) for i in range(NCC)]
        self.outt = [Tl(out.t[i * 128:(i + 1) * 128, :], "out%d" % i) for i in range(NCH)]

        self.cst = self.sb(root, "cst", [128, 8, 128])
        self.ident = Tl(self.cst.t[:, 0, :], "ident")
        self.ident.b = self.cst.b
        self.epsT = self.sb(root, "epsT", [128, 1])
        self.sT = self.sb(root, "sT", [128, 8, 2])
        self.ps = [Tl(root.enter_context(nc.psum_tensor("ps%d" % i, [128, 512], F32)), "ps%d" % i) for i in range(8)]
        for p_ in self.ps:
            p_.b.psum = True
        self.dma("sp", self.cst[:, :, :], I["consts"].t.rearrange("p (a b) -> p a b", b=128), [I["consts"]], [self.cst], self.cst)
        S.op("dve", lambda e: e.memset(self.epsT[:, :], EPS), [], [self.epsT])

        self.phase_mod()
        if self.stop == "mod":
            S.flush()
            return nc
        for l in range(DEPTH):
            self.phase_mixer(l)
            if self.stop == "mix%d" % l or self.stop in ("mixA", "mixB", "mixC"):
                break
            self.phase_moe(l)
            if self.stop == "moe%d" % l:
                break
        S.flush()
        return nc

    def phase_mod(self):
        S, I, ps = self.S, self.I, self.ps
        with contextlib.ExitStack() as st:
            cTs = self.sb(st, "cTs", [128, 8, 2])
            wa = [self.sb(st, "wa%d" % i, [128, 3072]) for i in range(2)]
            msb = self.sb(st, "msb", [2, 6 * D])
            m1p = self.sb(st, "m1p", [2, 6 * D])
            bad = self.sb(st, "bad", [2, 6 * D])
            self.dma("sp", cTs[:, :, :], I["cT"].t.rearrange("p (k m) -> p k m", m=2), [I["cT"]], [cTs], cTs)
            self.act(self.sT[:, :, :], cTs[:, :, :], AF.Silu, [cTs], [self.sT])
            it = 0
            for l in range(DEPTH):
                self.dma("sp", bad[:, :], I["b_ada"].t[l].partition_broadcast(2), [I["b_ada"]], [bad], bad)
                for half in range(2):
                    for kc in range(8):
                        w = wa[it % 2]
                        it += 1
                        self.dma("sp", w[:, :], I["w_ada"].t[l, kc * 128:(kc + 1) * 128, half * 3072:(half + 1) * 3072],
                                 [I["w_ada"]], [w], w)
                        for cb in range(6):
                            self.mm(ps[cb][0:2, :], self.sT[:, kc, :], w[:, cb * 512:(cb + 1) * 512], kc == 0, kc == 7,
                                    [self.sT, w], [ps[cb]])
                    for cb in range(6):
                        c0 = half * 3072 + cb * 512
                        self.tt("dve", msb[:, c0:c0 + 512], ps[cb][0:2, :], bad[:, c0:c0 + 512], ALU.add, [ps[cb], bad], [msb])
                self.ts("dve", m1p[:, :], msb[:, :], 1.0, None, ALU.add, None, [msb], [m1p])
                self.dma("sp", self.modd.t[l, :, 0, :], msb[:, :], [msb], [self.modd], msb)
                self.dma("sp", self.modd.t[l, :, 1, :], m1p[:, :], [m1p], [self.modd], m1p)
            S.flush()

    def phase_mixer(self, l):
        S, I, ps, nc = self.S, self.I, self.ps, self.nc
        last = l == DEPTH - 1
        with contextlib.ExitStack() as st:
            sb = lambda name, shape, dt=F32: self.sb(st, name, shape, dt)
            tmpst = contextlib.ExitStack()
            sbt = lambda name, shape, dt=F32: self.sb(tmpst, name, shape, dt)
            win = sb("win", [128, 8, IN_COLS], BF16)
            wout = sb("wout", [128, 8, D], BF16)
            for kc in range(8):
                self.dma("pool", win[:, kc, :], I["w_in"].t[l, kc * 128:(kc + 1) * 128, :], [I["w_in"]], [win], win,
                         max_dma_last_dim=8192)
            for kc in range(8):
                self.dma("pool", wout[:, kc, :], I["w_out"].t[l, kc * 128:(kc + 1) * 128, :], [I["w_out"]], [wout], wout,
                         max_dma_last_dim=8192)
            g1 = sb("g1", [128, D])
            lng = sb("lng", [128, D])
            lnb = sb("lnb", [128, D])
            self.dma("sp", lng[:, :], I["ln1_g"].t[l].partition_broadcast(128), [I["ln1_g"]], [lng], lng)
            self.dma("sp", lnb[:, :], I["ln1_b"].t[l].partition_broadcast(128), [I["ln1_b"]], [lnb], lnb)
            modT = sb("modT", [128, 2, 2, 8])
            for m in range(2):
                self.dma("sp", modT[:, m, 0, :], self.modd.t[l, m, 0, 0:1024].rearrange("(k p) -> p k", p=128),
                         [self.modd], [modT], modT, allow_slow_non_contiguous=True)
                self.dma("sp", modT[:, m, 1, :], self.modd.t[l, m, 1, 1024:2048].rearrange("(k p) -> p k", p=128),
                         [self.modd], [modT], modT, allow_slow_non_contiguous=True)
            dec = sb("dec", [128, 16])
            decq = sb("decq", [128, 8])
            self.dma("sp", dec[:, 0:8], I["dec_f"].t[l].partition_broadcast(128), [I["dec_f"]], [dec], dec)
            self.dma("sp", dec[:, 8:16], I["dec_b"].t[l].partition_broadcast(128), [I["dec_b"]], [dec], dec)
            for hp in range(2):
                for di, nm in enumerate(("dec_f", "dec_b")):
                    src = I[nm].t[l].rearrange("(c two) -> two c", two=2)[hp]
                    self.dma("sp", decq[hp * 64:(hp + 1) * 64, di * 4:(di + 1) * 4], src.partition_broadcast(64),
                             [I[nm]], [decq], decq, allow_slow_non_contiguous=True)
            lg = sb("lg", [128, 16])
            lgq = sb("lgq", [128, 8])
            for (src, dst) in ((dec, lg), (decq, lgq)):
                self.act(dst[:, :], src[:, :], AF.Exp, [src], [dst], scale=-1.0)
                self.act(dst[:, :], dst[:, :], AF.Ln, [dst], [dst], bias=1.0, scale=1.0)
                self.ts("dve", dst[:, :], dst[:, :], -1.0, None, ALU.mult, None, [dst], [dst])
            cst = self.cst
            dcomb = sb("dcomb", [128, 8, 128])
            tqm = sb("tqm", [128, 3, 2, 4, 128])
            pm8 = sb("pm8", [128, 2])
            kd = sb("kd", [128, 2, 8])
            cd = sb("cd", [128, 2, 4])
            wsT = sb("wsT", [128, 4, 128], BF16)
            bsT = sb("bsT", [128, 2, 128])
            cw = sb("cw", [128, 2, 3])
            zall = sb("zall", [128, 2, SEQ + 2 * ZPAD], BF16)
            sball = sb("sball", [128, NCH, 4, 64], BF16)
            st32 = [sb("st32_%d" % d_, [128, 4, 64]) for d_ in range(2)]
            sfbf = sb("sfbf", [128, 4, 64], BF16)
            stmp = sb("stmp", [128, 4, 64])
            s0 = [sb("s0_%d" % d_, [128, 4, 64]) for d_ in range(2)]
            dtmp = sbt("dtmp", [128, 128])
            dtmp2 = sbt("dtmp2", [128, 128])
            for h in range(8):
                self.act(dtmp[:, :], cst[:, 1, :], AF.Exp, [cst, lg], [dtmp], scale=lg[:, h:h + 1])
                self.tt("dve", dtmp[:, :], dtmp[:, :], cst[:, 2, :], ALU.mult, [dtmp, cst], [dtmp])
                self.act(dtmp2[:, :], cst[:, 3, :], AF.Exp, [cst, lg], [dtmp2], scale=lg[:, 8 + h:9 + h])
                self.tt("dve", dtmp2[:, :], dtmp2[:, :], cst[:, 4, :], ALU.mult, [dtmp2, cst], [dtmp2])
                self.tt("dve", dcomb[:, h, :], dtmp[:, :], dtmp2[:, :], ALU.add, [dtmp, dtmp2], [dcomb])
            tq = sbt("tq", [128, 2, 4, 128])
            for c in range(4):
                self.act(tq[:, 0, c, :], cst[:, 5, :], AF.Exp, [cst, lgq], [tq], scale=lgq[:, c:c + 1])
                self.act(tq[:, 1, c, :], cst[:, 6, :], AF.Exp, [cst, lgq], [tq], scale=lgq[:, 4 + c:5 + c])
            self.ts("dve", tq[:, :, :, :], tq[:, :, :, :], 0.125, None, ALU.mult, None, [tq], [tq])
            self.ts("dve", pm8[:, :], cst[:, 7, 2:4], 0.125, None, ALU.mult, None, [cst], [pm8])
            for hp in range(2):
                self.ts("dve", tqm[:, 0, hp, :, :], cst[:, 5, :].unsqueeze(1).to_broadcast([128, 4, 128]), 0.0, pm8[:, hp:hp + 1],
                        ALU.mult, ALU.add, [cst, pm8], [tqm])
                for var in range(2):
                    self.ts("dve", tqm[:, 1 + var, hp, :, :], tq[:, var, :, :], cst[:, 7, 2 + hp:3 + hp], None, ALU.mult, None,
                            [tq, cst], [tqm])
            self.act(kd[:, 0, :], lg[:, 0:8], AF.Exp, [lg, cst], [kd], scale=cst[:, 7, 0:1])
            self.act(kd[:, 1, :], lg[:, 8:16], AF.Exp, [lg, cst], [kd], scale=cst[:, 7, 1:2])
            self.act(cd[:, 0, :], lgq[:, 0:4], AF.Exp, [lgq], [cd], scale=128.0)
            self.act(cd[:, 1, :], lgq[:, 4:8], AF.Exp, [lgq], [cd], scale=128.0)
            wsn = sbt("wsn", [128, 4, 128])
            self.dma("sp", wsn[:, :, :], I["sgu_w"].t[l].rearrange("g p q -> p g q"), [I["sgu_w"]], [wsn], wsn)
            for g in range(4):
                self.tr(ps[0][:, g * 128:(g + 1) * 128], wsn[:, g, :], [wsn], [ps[0]])
            self.cp("act", wsT[:, :, :], ps[0][:, :].rearrange("p (g q) -> p g q", q=128), [ps[0]], [wsT])
            for gp in range(2):
                for gi in range(2):
                    self.dma("sp", bsT[gi * 64:(gi + 1) * 64, gp, :], I["sgu_b"].t[l, 2 * gp + gi].partition_broadcast(64),
                             [I["sgu_b"]], [bsT], bsT)
            for c in range(2):
                for k in range(3):
                    self.dma("sp", cw[:, c, k:k + 1], I["conv_w"].t[l, k, c * 128:(c + 1) * 128].rearrange("(p o) -> p o", o=1),
                             [I["conv_w"]], [cw], cw)
            W = dict(win=win, wout=wout, g1=g1, lng=lng, lnb=lnb, modT=modT, dcomb=dcomb, tq=tq, tqm=tqm, kd=kd, cd=cd,
                     wsT=wsT, bsT=bsT, cw=cw, zall=zall, sball=sball, st32=st32, sfbf=sfbf, stmp=stmp)
            S.flush()
            tmpst.close()
            wk = {}
            wk["xt"] = [sb("xt%d" % i, [128, D]) for i in range(2)]
            wk["hT"] = [sb("hT%d" % i, [128, 8, 128], BF16) for i in range(2)]
            one = dict(ktok=sb("ktok", [128, 512], BF16), vbf=sb("vbf", [128, 512], BF16), vd=sb("vd", [128, 512], BF16),
                       q8T=sb("q8T", [128, 4, 128]), qm=sb("qm", [128, 6, 4, 128], BF16), kT=sb("kT", [128, 4, 128], BF16))
            wk["A"] = [dict(one, sg=sb("sg%d" % i, [128, 512]), cbs=sb("cbs%d" % i, [128, 2, 128]),
                            uT=sb("uT%d" % i, [128, 2, 128]), vn=sb("vn%d" % i, [128, 256], BF16)) for i in range(2)]
            wk["ktok"], wk["vd"], wk["vbf"] = wk["A"][0]["ktok"], wk["A"][0]["vd"], wk["A"][0]["vbf"]
            wk["msk"] = sb("msk", [128, 8, 128], BF16)
            wk["osb"] = sb("osb", [128, 512])
            wk["gst"] = sb("gst", [128, 4, 8])
            wk["yb"] = sb("yb", [128, 512])
            wk["mixT"] = sb("mixT", [128, 8, 128], BF16)
            wk["vst"] = sb("vst", [128, 16])
            wk["cv"] = sb("cv", [128, 2, 128])
            wk["mx"] = sb("mx", [128, 2, 128])
            wk["cxs"] = wk["mx"]
            wk["xo"] = [sb("xo%d" % i, [128, D]) for i in range(2)]
            wk["lnsc"] = dict(st=sb("lnst", [128, 12]), mv=sb("lnmv", [128, 2]), rs=sb("lnrs", [128, 2]))
            self.wk = wk
            self.W = W
            self.itx = 0
            zero = lambda eng, t: S.op(eng, lambda e: e.memset(t[:], 0.0), [], [t])
            src_c = self.cin if l == 0 else self.c2[l - 1]
            self.dma("sp", g1[:, :], self.modd.t[l, 1, 0, 2048:3072].partition_broadcast(128), [self.modd], [g1], g1)
            if self.stop == "mixA":
                S.flush()
                return
            zero("pool", zall)
            zero("dve", st32[0])
            zero("dve", st32[1])
            self.prepass(l, 1, src_c, NCC, fwd_final=last)
            if self.stop == "mixB":
                S.flush()
                return
            if not last:
                zero("dve", st32[0])
                self.mainpass(l, 1, src_c, self.c1[l], NCC, grid=False)
            if self.stop == "mixC":
                S.flush()
                return
            for d_ in range(2):
                self.cp("dve", s0[d_][:, :, :], st32[d_][:, :, :], [st32[d_]], [s0[d_]])
            if self.debug:
                self.dbg_states = self.dram_tmp("dbgst%d" % l, [2, 128, 256])
                for d_ in range(2):
                    self.dma("sp", self.dbg_states.t[d_], s0[d_][:, :, :].rearrange("p c e -> p (c e)"), [s0[d_]], [self.dbg_states], s0[d_])
            src_x = self.xin if l == 0 else self.x2[l - 1]
            self.dma("sp", g1[:, :], self.modd.t[l, 0, 0, 2048:3072].partition_broadcast(128), [self.modd], [g1], g1)
            zero("pool", zall)
            self.prepass(l, 0, src_x, NCH, fwd_final=False)
            self.cp("dve", st32[0][:, :, :], s0[0][:, :, :], [s0[0]], [st32[0]])
            self.mainpass(l, 0, src_x, self.x1[l], NCH, grid=True)
            S.flush()

    def load_hT(self, l, m, src, n):
        S, ps, wk, W = self.S, self.ps, self.wk, self.W
        i = self.itx
        self.itx += 1
        xt = wk["xt"][i % 2]
        hT = wk["hT"][i % 2]
        self.dma("sp", xt[:, :], src[n][:, :], [src[n]], [xt], xt)
        for kc in range(8):
            self.tr(ps[kc // 4][:, (kc % 4) * 128:(kc % 4 + 1) * 128], xt[:, kc * 128:(kc + 1) * 128], [xt], [ps[kc // 4]])
        modT = W["modT"]
        for kc in range(8):
            src_ps = ps[kc // 4][:, (kc % 4) * 128:(kc % 4 + 1) * 128]
            if kc < 4:
                self.act(hT[:, kc, :], src_ps, AF.Identity, [ps[kc // 4], modT], [hT],
                         bias=modT[:, m, 0, kc:kc + 1], scale=modT[:, m, 1, kc:kc + 1])
            else:
                self.ts("dve", hT[:, kc, :], src_ps, modT[:, m, 1, kc:kc + 1], modT[:, m, 0, kc:kc + 1], ALU.mult, ALU.add,
                        [ps[kc // 4], modT], [hT])
        return xt, hT

    def proj_fm(self, hT, bank, slot, col0):
        win = self.W["win"]
        for kc in range(8):
            self.mm(bank[:, slot * 128:(slot + 1) * 128], win[:, kc, col0:col0 + 128], hT[:, kc, :], kc == 0, kc == 7,
                    [win, hT], [bank])

    def proj_tm(self, hT, bank, col0, ncols):
        win = self.W["win"]
        for kc in range(8):
            self.mm(bank[:, 0:ncols], hT[:, kc, :], win[:, kc, col0:col0 + ncols], kc == 0, kc == 7, [win, hT], [bank])

    def state_update(self, d_, bank, ktok, vd, dst_bf, fwd_weight=None):
        S, W = self.S, self.W
        st = W["st32"][d_]
        stmp = W["stmp"]
        cd = W["cd"]
        for c in range(4):
            self.mm(bank[:, c * 128:(c + 1) * 128], ktok[:, c * 128:(c + 1) * 128], vd[:, c * 128:(c + 1) * 128], True, True,
                    [ktok, vd], [bank])
        bv = bank[:, :].rearrange("p (c x) -> p c x", x=128)
        if fwd_weight is None:
            self.tt("pool", stmp[:, :, :], st[:, :, :], cd[:, d_, :].unsqueeze(2).to_broadcast([128, 4, 64]), ALU.mult, [st, cd], [stmp])
            for hp in range(2):
                p0, p1 = hp * 64, hp * 64 + 64
                self.tt("dve", st[p0:p1, :, :], stmp[p0:p1, :, :], bv[p0:p1, :, hp * 64:hp * 64 + 64], ALU.add, [stmp, bank], [st])
        else:
            for hp in range(2):
                p0, p1 = hp * 64, hp * 64 + 64
                if fwd_weight == "one":
                    self.tt("dve", st[p0:p1, :, :], st[p0:p1, :, :], bv[p0:p1, :, hp * 64:hp * 64 + 64], ALU.add, [st, bank], [st])
                else:
                    self.tt("dve", stmp[p0:p1, :, :], bv[p0:p1, :, hp * 64:hp * 64 + 64],
                            cd[p0:p1, d_, :].unsqueeze(2).to_broadcast([64, 4, 64]), ALU.mult, [bank, cd], [stmp])
                    self.tt("dve", st[p0:p1, :, :], st[p0:p1, :, :], stmp[p0:p1, :, :], ALU.add, [st, stmp], [st])
        if dst_bf is not None:
            ap, tl = dst_bf
            self.cp("act", ap, st[:, :, :], [st], [tl])

    def prepass(self, l, m, src, N, fwd_final):
        S, ps, wk, W = self.S, self.ps, self.wk, self.W
        zall, sball, kd = W["zall"], W["sball"], W["kd"]
        st32 = W["st32"]
        self.cp("act", sball[:, N - 1, :, :], st32[1][:, :, :], [st32[1]], [sball])
        for n in range(N - 1, -1, -1):
            xt, hT = self.load_hT(l, m, src, n)
            for j, col in enumerate((O_CC, O_CC + 128, O_CX, O_CX + 128)):
                self.proj_fm(hT, ps[2], j, col)
            self.proj_tm(hT, ps[3], O_K, 512)
            self.proj_tm(hT, ps[4], O_V, 512)
            cxs = wk["cxs"]
            self.cp("act", cxs[:, :, :], ps[2][:, 256:512].rearrange("p (c t) -> p c t", t=128), [ps[2]], [cxs])
            self.tt("dve", zall[:, :, ZPAD + n * 128:ZPAD + (n + 1) * 128], ps[2][:, 0:256].rearrange("p (c t) -> p c t", t=128),
                    cxs[:, :, :], ALU.mult, [ps[2], cxs], [zall])
            ktok, vd = wk["ktok"], wk["vd"]
            self.cp("act", ktok[:, :], ps[3][:, :], [ps[3]], [ktok])
            self.tt("dve", vd[:, :].rearrange("p (h e) -> p h e", e=64), ps[4][:, :].rearrange("p (h e) -> p h e", e=64),
                    kd[:, 1, :].unsqueeze(2).to_broadcast([128, 8, 64]), ALU.mult, [ps[4], kd], [vd])
            dst = (sball[:, n - 1, :, :], sball) if n >= 1 else None
            self.state_update(1, ps[5], ktok, vd, dst)
            if fwd_final:
                vdf = wk["vbf"]
                self.tt("dve", vdf[:, :].rearrange("p (h e) -> p h e", e=64), ps[4][:, :].rearrange("p (h e) -> p h e", e=64),
                        kd[:, 0, :].unsqueeze(2).to_broadcast([128, 8, 64]), ALU.mult, [ps[4], kd], [vdf])
                assert N == 2
                self.state_update(0, ps[6], ktok, vdf, None, fwd_weight=("one" if n == N - 1 else "cd"))

    def stageA(self, l, m, src, n, sset):
        S, ps, wk, W = self.S, self.ps, self.wk, self.W
        kd, tqm = W["kd"], W["tqm"]
        A = wk["A"][sset]
        xt, hT = self.load_hT(l, m, src, n)
        A["xt"] = xt
        q8T, qm, kT, vbf, vd, ktok, sg, cbs, uT, vn = (A[k] for k in ("q8T", "qm", "kT", "vbf", "vd", "ktok", "sg", "cbs", "uT", "vn"))
        vst = wk["vst"]
        for c in range(4):
            self.proj_fm(hT, ps[2], c, O_Q + c * 128)
        for c in range(4):
            self.proj_fm(hT, ps[3], c, O_K + c * 128)
        self.proj_tm(hT, ps[0], O_V, 512)
        self.proj_tm(hT, ps[1], O_G, 512)
        if SUB == 1:
            return
        self.cp("act", q8T[:, :, :], ps[2][:, :].rearrange("p (c t) -> p c t", t=128), [ps[2]], [q8T])
        self.cp("act", kT[:, :, :], ps[3][:, :].rearrange("p (c t) -> p c t", t=128), [ps[3]], [kT])
        self.cp("act", vbf[:, :], ps[0][:, :], [ps[0]], [vbf])
        if SUB == 11:
            return
        self.tt("dve", vd[:, :].rearrange("p (h e) -> p h e", e=64), ps[0][:, :].rearrange("p (h e) -> p h e", e=64),
                kd[:, 0, :].unsqueeze(2).to_broadcast([128, 8, 64]), ALU.mult, [ps[0], kd, vbf], [vd])
        if SUB == 12:
            return
        self.sigmoid(sg[:, :], ps[1][:, :], [ps[1]], [sg])
        self.tt("dve", sg[:, :], sg[:, :], ps[1][:, :], ALU.mult, [sg, ps[1]], [sg])
        if SUB == 13:
            return
        for vh in range(6):
            self.tt("dve", qm[:, vh, :, :], q8T[:, :, :], tqm[:, vh // 2, vh % 2, :, :], ALU.mult, [q8T, tqm], [qm])
        if SUB == 2:
            return
        for j, col in enumerate((O_CB, O_CB + 128, O_U, O_U + 128)):
            self.proj_fm(hT, ps[2], j, col)
        self.proj_tm(hT, ps[3], O_K, 512)
        self.proj_tm(hT, ps[0], O_VV, 256)
        if SUB == 3:
            return
        self.cp("act", cbs[:, :, :], ps[2][:, 0:256].rearrange("p (c t) -> p c t", t=128), [ps[2]], [cbs])
        self.cp("act", uT[:, :, :], ps[2][:, 256:512].rearrange("p (c t) -> p c t", t=128), [ps[2]], [uT])
        self.cp("act", ktok[:, :], ps[3][:, :], [ps[3]], [ktok])
        S.op("dve", lambda e: e.bn_stats(vst[:, 0:6], ps[0][:, 0:256]), [ps[0]], [vst])
        S.op("dve", lambda e: e.bn_aggr(vst[:, 6:8], vst[:, 0:6]), [vst], [vst])
        self.act(vst[:, 8:9], vst[:, 7:8], AF.Ln, [vst, self.epsT], [vst], bias=self.epsT[:, 0:1], scale=1.0)
        self.act(vst[:, 9:10], vst[:, 8:9], AF.Exp, [vst], [vst], scale=-0.5)
        self.ts("dve", vn[:, :], ps[0][:, 0:256], vst[:, 6:7], vst[:, 9:10], ALU.subtract, ALU.mult, [ps[0], vst], [vn])

    def stageB1(self, n, sset):
        S, ps, wk, W = self.S, self.ps, self.wk, self.W
        sball, dcomb, st32, sfbf = W["sball"], W["dcomb"], W["st32"], W["sfbf"]
        A = wk["A"][sset]
        qm, kT, vbf, vd, ktok, sg = A["qm"], A["kT"], A["vbf"], A["vd"], A["ktok"], A["sg"]
        msk = wk["msk"]
        for h in range(8):
            c, hp = h // 2, h % 2
            bank = ps[4 + h // 4]
            self.mm(bank[:, (h % 4) * 128:(h % 4 + 1) * 128], kT[:, c, :], qm[:, 0 + hp, c, :], True, True, [kT, qm], [bank])
        for b2 in range(2):
            self.tt("dve", msk[:, b2 * 4:(b2 + 1) * 4, :], ps[4 + b2][:, :].rearrange("p (h t) -> p h t", t=128),
                    dcomb[:, b2 * 4:(b2 + 1) * 4, :], ALU.mult, [ps[4 + b2], dcomb], [msk])
        for h in range(8):
            c, hp = h // 2, h % 2
            o_ap = ps[6][:, h * 64:(h + 1) * 64]
            self.mm(o_ap, msk[:, h, :], vbf[:, h * 64:(h + 1) * 64], True, False, [msk, vbf], [ps[6]])
            self.mm(o_ap, qm[:, 2 + hp, c, :], sfbf[:, c, :], False, False, [qm, sfbf], [ps[6]])
            self.mm(o_ap, qm[:, 4 + hp, c, :], sball[:, n, c, :], False, True, [qm, sball], [ps[6]])
        self.state_update(0, ps[7], ktok, vd, (sfbf[:, :, :], sfbf))
        osb, gst, yb = wk["osb"], wk["gst"], wk["yb"]
        self.cp("act", osb[:, :], ps[6][:, :], [ps[6]], [osb])
        self.tt("dve", yb[:, :], osb[:, :], osb[:, :], ALU.mult, [osb], [yb])
        self.red(gst[:, 0, :], osb[:, :].rearrange("p (h e) -> p h e", e=64), ALU.add, [osb], [gst])
        self.red(gst[:, 1, :], yb[:, :].rearrange("p (h e) -> p h e", e=64), ALU.add, [yb], [gst])
        self.ts("dve", gst[:, 0:2, :], gst[:, 0:2, :], 1.0 / 64, None, ALU.mult, None, [gst], [gst])
        self.tt("dve", gst[:, 2, :], gst[:, 0, :], gst[:, 0, :], ALU.mult, [gst], [gst])
        self.tt("dve", gst[:, 1, :], gst[:, 1, :], gst[:, 2, :], ALU.subtract, [gst], [gst])
        self.act(gst[:, 2, :], gst[:, 1, :], AF.Ln, [gst, self.epsT], [gst], bias=self.epsT[:, 0:1], scale=1.0)
        self.act(gst[:, 3, :], gst[:, 2, :], AF.Exp, [gst], [gst], scale=-0.5)
        o3 = osb[:, :].rearrange("p (h e) -> p h e", e=64)
        self.tt("dve", o3, o3, gst[:, 0, :].unsqueeze(2).to_broadcast([128, 8, 64]), ALU.subtract, [osb, gst], [osb])
        self.tt("dve", o3, o3, gst[:, 3, :].unsqueeze(2).to_broadcast([128, 8, 64]), ALU.mult, [osb, gst], [osb])
        self.tt("dve", yb[:, :], osb[:, :], sg[:, :], ALU.mult, [osb, sg], [yb])

    def stageB2(self, m, dst, n, sset, grid):
        S, ps, wk, W = self.S, self.ps, self.wk, self.W
        wout, zall = W["wout"], W["zall"]
        A = wk["A"][sset]
        xt, cbs, uT, vn = A["xt"], A["cbs"], A["uT"], A["vn"]
        yb, mixT = wk["yb"], wk["mixT"]
        for c in range(4):
            self.tr(ps[4][:, c * 128:(c + 1) * 128], yb[:, c * 128:(c + 1) * 128], [yb], [ps[4]])
        self.cp("act", mixT[:, 2:6, :], ps[4][:, :].rearrange("p (c t) -> p c t", t=128), [ps[4]], [mixT])
        wsT, bsT = W["wsT"], W["bsT"]
        for g in range(4):
            gp = g // 2
            self.mm(ps[5][:, g * 128:(g + 1) * 128], vn[:, gp * 128:(gp + 1) * 128], wsT[:, g, :], True, True, [vn, wsT], [ps[5]])
        mx = wk["mx"]
        for g in range(4):
            gp, gi = g // 2, g % 2
            self.tt("dve", mx[gi * 64:(gi + 1) * 64, gp, :], ps[5][gi * 64:(gi + 1) * 64, g * 128:(g + 1) * 128],
                    bsT[gi * 64:(gi + 1) * 64, gp, :], ALU.add, [ps[5], bsT], [mx])
        self.tt("dve", mixT[:, 6:8, :], mx[:, :, :], uT[:, :, :], ALU.mult, [mx, uT], [mixT])
        cv, cw = wk["cv"], W["cw"]
        t0 = ZPAD + n * 128
        if grid:
            z0 = zall[:, 0, t0:t0 + 128].rearrange("p (r w) -> p r w", w=64)
            c0 = cv[:, 0, :].rearrange("p (r w) -> p r w", w=64)
            self.ts("dve", cv[:, 0, :], zall[:, 0, t0:t0 + 128], cw[:, 0, 1:2], None, ALU.mult, None, [zall, cw], [cv])
            self.stt(c0[:, :, 1:64], z0[:, :, 0:63], cw[:, 0, 0:1], c0[:, :, 1:64], ALU.mult, ALU.add, [zall, cw, cv], [cv])
            self.stt(c0[:, :, 0:63], z0[:, :, 1:64], cw[:, 0, 2:3], c0[:, :, 0:63], ALU.mult, ALU.add, [zall, cw, cv], [cv])
            self.ts("dve", cv[:, 1, :], zall[:, 1, t0:t0 + 128], cw[:, 1, 1:2], None, ALU.mult, None, [zall, cw], [cv])
            self.stt(cv[:, 1, :], zall[:, 1, t0 - 64:t0 + 64], cw[:, 1, 0:1], cv[:, 1, :], ALU.mult, ALU.add, [zall, cw, cv], [cv])
            self.stt(cv[:, 1, :], zall[:, 1, t0 + 64:t0 + 192], cw[:, 1, 2:3], cv[:, 1, :], ALU.mult, ALU.add, [zall, cw, cv], [cv])
        else:
            for ch in range(2):
                self.ts("dve", cv[:, ch, :], zall[:, ch, t0:t0 + 128], cw[:, ch, 1:2], None, ALU.mult, None, [zall, cw], [cv])
                self.stt(cv[:, ch, :], zall[:, ch, t0 - 1:t0 + 127], cw[:, ch, 0:1], cv[:, ch, :], ALU.mult, ALU.add, [zall, cw, cv], [cv])
                self.stt(cv[:, ch, :], zall[:, ch, t0 + 1:t0 + 129], cw[:, ch, 2:3], cv[:, ch, :], ALU.mult, ALU.add, [zall, cw, cv], [cv])
        self.tt("dve", mixT[:, 0:2, :], cv[:, :, :], cbs[:, :, :], ALU.mult, [cv, cbs], [mixT])
        for hh, bank in ((0, ps[6]), (1, ps[7])):
            for kc in range(8):
                self.mm(bank[:, :], mixT[:, kc, :], wout[:, kc, hh * 512:(hh + 1) * 512], kc == 0, kc == 7, [mixT, wout], [bank])
        xo = wk["xo"][n % 2]
        r, g1 = xo, W["g1"]
        for hh, bank in ((0, ps[6]), (1, ps[7])):
            self.tt("dve", r[:, hh * 512:(hh + 1) * 512], bank[:, :], g1[:, hh * 512:(hh + 1) * 512], ALU.mult, [bank, g1], [r])
        self.stt(r[:, :], xt[:, :], ALPHA, r[:, :], ALU.mult, ALU.add, [xt, r], [r])
        self.layernorm(r, W["lng"], W["lnb"], xo, wk["lnsc"])
        self.dma("sp", dst[n][:, :], xo[:, :], [xo], [dst[n]], xo)

    def mainpass(self, l, m, src, dst, N, grid):
        W = self.W
        st32, sfbf = W["st32"], W["sfbf"]
        self.cp("act", sfbf[:, :, :], st32[0][:, :, :], [st32[0]], [sfbf])
        self.stageA(l, m, src, 0, 0)
        if CUT == 1:
            return
        for n in range(N):
            self.stageB1(n, n % 2)
            if CUT == 2:
                return
            if n + 1 < N:
                self.stageA(l, m, src, n + 1, (n + 1) % 2)
            if CUT == 3:
                return
            self.stageB2(m, dst, n, n % 2, grid)
            if CUT == 4:
                return

    def phase_moe(self, l):
        S, I, ps, nc = self.S, self.I, self.ps, self.nc
        last = l == DEPTH - 1
        tiles = []
        if not last:
            for n in range(NCC):
                tiles.append((1, self.c1[l][n], self.c2[l][n]))
        for n in range(NCH):
            tiles.append((0, self.x1[l][n], self.outt[n] if last else self.x2[l][n]))
        ngrp = 3
        per = -(-len(tiles) // ngrp)
        groups = [tiles[i * per:(i + 1) * per] for i in range(ngrp)]
        GMAX = per
        with contextlib.ExitStack() as st:
            sb = lambda name, shape, dt=F32: self.sb(st, name, shape, dt)
            g2 = [sb("g2_%d" % m, [128, D]) for m in range(2)]
            lng = sb("lng2", [128, D])
            lnb = sb("lnb2", [128, D])
            for m in range(2):
                self.dma("sp", g2[m][:, :], self.modd.t[l, m, 0, 5120:6144].partition_broadcast(128), [self.modd], [g2[m]], g2[m])
            self.dma("sp", lng[:, :], I["ln2_g"].t[l].partition_broadcast(128), [I["ln2_g"]], [lng], lng)
            self.dma("sp", lnb[:, :], I["ln2_b"].t[l].partition_broadcast(128), [I["ln2_b"]], [lnb], lnb)
            modT = sb("modT2", [128, 2, 2, 8])
            for m in range(2):
                self.dma("sp", modT[:, m, 0, :], self.modd.t[l, m, 0, 3072:4096].rearrange("(k p) -> p k", p=128),
                         [self.modd], [modT], modT, allow_slow_non_contiguous=True)
                self.dma("sp", modT[:, m, 1, :], self.modd.t[l, m, 1, 4096:5120].rearrange("(k p) -> p k", p=128),
                         [self.modd], [modT], modT, allow_slow_non_contiguous=True)
            wr = sb("wr", [128, 8, 36])
            brt = sb("brt", [128, 36])
            self.dma("sp", wr[:, :, :], I["wr"].t[l].rearrange("(k p) c -> p k c", p=128), [I["wr"]], [wr], wr)
            self.dma("sp", brt[:, :], I["br"].t[l].partition_broadcast(128), [I["br"]], [brt], brt)
            yacc = [sb("yacc%d" % i, [128, D]) for i in range(GMAX)]
            hmT = [sb("hmT%d" % i, [128, 8, 512], BF16) for i in range((GMAX + 3) // 4)]
            gate = [sb("gate%d" % i, [128, 32]) for i in range(GMAX)]
            hmf = sb("hmf", [128, 8, 128])
            wgs = [sb("wgs%d" % i, [128, 8, EH], BF16) for i in range(2)]
            wus = [sb("wus%d" % i, [128, 8, EH], BF16) for i in range(2)]
            wds = [sb("wds%d" % i, [128, 4, D], BF16) for i in range(2)]
            xt = [sb("mxt%d" % i, [128, D]) for i in range(2)]
            xo = [sb("mxo%d" % i, [128, D]) for i in range(2)]
            r = sb("mr", [128, D])
            sgs = [sb("sgs%d" % i, [128, 512]) for i in range(2)]
            hT = [sb("hhT%d" % i, [128, 4, 512], BF16) for i in range(2)]
            rt = sb("rt", [128, 128])
            lnsc = dict(st=sb("lnst2", [128, 12]), mv=sb("lnmv2", [128, 2]), rs=sb("lnrs2", [128, 2]))
            wcount = 0
            hcount = 0
            for grp in groups:
                G = len(grp)
                for ti, (m, src, dst) in enumerate(grp):
                    x_t = xt[ti % 2]
                    self.dma("sp", x_t[:, :], src[:, :], [src], [x_t], x_t)
                    for kc in range(8):
                        self.tr(ps[kc // 4][:, (kc % 4) * 128:(kc % 4 + 1) * 128], x_t[:, kc * 128:(kc + 1) * 128], [x_t], [ps[kc // 4]])
                    for kc in range(8):
                        src_ps = ps[kc // 4][:, (kc % 4) * 128:(kc % 4 + 1) * 128]
                        self.act(hmf[:, kc, :], src_ps, AF.Identity, [ps[kc // 4], modT], [hmf],
                                 bias=modT[:, m, 0, kc:kc + 1], scale=modT[:, m, 1, kc:kc + 1])
                    self.cp("dve", hmT[ti // 4][:, :, (ti % 4) * 128:(ti % 4 + 1) * 128], hmf[:, :, :], [hmf], [hmT[ti // 4]])
                    for kc in range(8):
                        self.mm(ps[2][:, 0:36], hmf[:, kc, :], wr[:, kc, :], kc == 0, kc == 7, [hmf, wr], [ps[2]])
                    self.tt("dve", rt[:, 0:36], ps[2][:, 0:36], brt[:, :], ALU.add, [ps[2], brt], [rt])
                    self.red(rt[:, 40:41], rt[:, 0:4], ALU.max, [rt], [rt])
                    self.ts("dve", rt[:, 41:42], rt[:, 40:41], -1.0, None, ALU.mult, None, [rt], [rt])
                    self.act(rt[:, 44:48], rt[:, 0:4], AF.Exp, [rt], [rt], bias=rt[:, 41:42], scale=1.0)
                    self.red(rt[:, 42:43], rt[:, 44:48], ALU.add, [rt], [rt])
                    S.op("dve", lambda e: e.reciprocal(rt[:, 43:44], rt[:, 42:43]), [rt], [rt])
                    self.ts("dve", rt[:, 48:52], rt[:, 0:4], rt[:, 40:41], None, ALU.is_equal, None, [rt], [rt])
                    self.tt("dve", rt[:, 64:96].rearrange("p (g e) -> p g e", e=8), rt[:, 4:36].rearrange("p (g e) -> p g e", e=8),
                            rt[:, 48:52].unsqueeze(2).to_broadcast([128, 4, 8]), ALU.mult, [rt], [rt])
                    self.red(rt[:, 96:104], rt[:, 64:96].rearrange("p (g e) -> p e g", e=8), ALU.add, [rt], [rt])
                    S.op("dve", lambda e: e.max(rt[:, 104:112], rt[:, 96:104]), [rt], [rt])
                    self.ts("dve", rt[:, 112:120], rt[:, 96:104], rt[:, 105:106], None, ALU.is_ge, None, [rt], [rt])
                    self.ts("dve", rt[:, 52:53], rt[:, 104:105], -1.0, None, ALU.mult, None, [rt], [rt])
                    self.act(rt[:, 120:128], rt[:, 96:104], AF.Exp, [rt], [rt], bias=rt[:, 52:53], scale=1.0)
                    self.tt("dve", rt[:, 120:128], rt[:, 120:128], rt[:, 112:120], ALU.mult, [rt], [rt])
                    self.red(rt[:, 53:54], rt[:, 120:128], ALU.add, [rt], [rt])
                    S.op("dve", lambda e: e.reciprocal(rt[:, 54:55], rt[:, 53:54]), [rt], [rt])
                    self.tt("dve", rt[:, 54:55], rt[:, 54:55], rt[:, 43:44], ALU.mult, [rt], [rt])
                    self.ts("dve", rt[:, 120:128], rt[:, 120:128], rt[:, 54:55], None, ALU.mult, None, [rt], [rt])
                    self.tt("dve", gate[ti][:, :].rearrange("p (g e) -> p g e", e=8),
                            rt[:, 48:52].unsqueeze(2).to_broadcast([128, 4, 8]),
                            rt[:, 120:128].unsqueeze(1).to_broadcast([128, 4, 8]), ALU.mult, [rt], [gate[ti]])
                mts = [list(range(i, min(i + 4, G))) for i in range(0, G, 4)]
                for e_ in range(NE):
                    wg, wu, wd = wgs[wcount % 2], wus[wcount % 2], wds[wcount % 2]
                    wcount += 1
                    for k4 in range(2):
                        self.dma("pool", wg[:, k4 * 4:(k4 + 1) * 4, :],
                                 I["wg"].t[l, e_, k4 * 512:(k4 + 1) * 512, :].rearrange("(k p) c -> p k c", p=128),
                                 [I["wg"]], [wg], wg)
                        self.dma("pool", wu[:, k4 * 4:(k4 + 1) * 4, :],
                                 I["wu"].t[l, e_, k4 * 512:(k4 + 1) * 512, :].rearrange("(k p) c -> p k c", p=128),
                                 [I["wu"]], [wu], wu)
                    self.dma("pool", wd[:, :, :], I["wd"].t[l, e_].rearrange("(k p) c -> p k c", p=128), [I["wd"]], [wd], wd)
                    for mt in mts:
                        nt = len(mt)
                        hTt = hT[hcount % 2]
                        hcount += 1
                        for hc in range(4):
                            pg, pu = ps[hc % 2], ps[2 + hc % 2]
                            for (bank, w_) in ((pg, wg), (pu, wu)):
                                for kc in range(8):
                                    self.mm(bank[:, 0:nt * 128], w_[:, kc, hc * 128:(hc + 1) * 128], hmT[mt[0] // 4][:, kc, 0:nt * 128],
                                            kc == 0, kc == 7, [w_, hmT[mt[0] // 4]], [bank])
                            sg_ = sgs[hc % 2]
                            self.sigmoid(sg_[:, 0:nt * 128], pg[:, 0:nt * 128], [pg], [sg_])
                            self.tt("dve", sg_[:, 0:nt * 128], sg_[:, 0:nt * 128], pg[:, 0:nt * 128], ALU.mult, [sg_, pg], [sg_])
                            self.tt("dve", hTt[:, hc, 0:nt * 128], sg_[:, 0:nt * 128], pu[:, 0:nt * 128], ALU.mult, [sg_, pu], [hTt])
                        for j, ti in enumerate(mt):
                            for hh in range(2):
                                bank = ps[4 + 2 * (j % 2) + hh]
                                for hc in range(4):
                                    self.mm(bank[:, :], hTt[:, hc, j * 128:(j + 1) * 128], wd[:, hc, hh * 512:(hh + 1) * 512], hc == 0, hc == 3,
                                            [hTt, wd], [bank])
                                ya = yacc[ti][:, hh * 512:(hh + 1) * 512]
                                if e_ == 0:
                                    self.ts("dve", ya, bank[:, :], gate[ti][:, 0:1], None, ALU.mult, None, [bank, gate[ti]], [yacc[ti]])
                                else:
                                    self.stt(ya, bank[:, :], gate[ti][:, e_:e_ + 1], ya, ALU.mult, ALU.add, [bank, gate[ti], yacc[ti]], [yacc[ti]])
                for ti, (m, src, dst) in enumerate(grp):
                    x_t = xt[ti % 2]
                    self.dma("sp", x_t[:, :], src[:, :], [src], [x_t], x_t)
                    self.tt("pool", r[:, :], yacc[ti][:, :], g2[m][:, :], ALU.mult, [yacc[ti], g2[m]], [r])
                    self.stt(r[:, :], x_t[:, :], ALPHA, r[:, :], ALU.mult, ALU.add, [x_t, r], [r])
                    xo_ = xo[ti % 2]
                    self.layernorm(r, lng, lnb, xo_, lnsc)
                    self.dma("sp", dst[:, :], xo_[:, :], [xo_], [dst], xo_)
            S.flush()


def _consts():
    c = np.zeros((128, 8, 128), np.float32)
    j = np.arange(128)[:, None].astype(np.float32)
    i = np.arange(128)[None, :].astype(np.float32)
    c[:, 0, :] = np.eye(128, dtype=np.float32)
    c[:, 1, :] = np.maximum(i - j, 0)
    c[:, 2, :] = (i >= j)
    c[:, 3, :] = np.maximum(j - i, 0)
    c[:, 4, :] = (j >= i)
    c[:, 5, :] = i + 1.0
    c[:, 6, :] = 128.0 - i
    c[:, 7, 0] = 127.0 - j[:, 0]
    c[:, 7, 1] = j[:, 0]
    c[:64, 7, 2] = 1.0
    c[64:, 7, 3] = 1.0
    return c.reshape(128, 8 * 128)


def make_in_maps(inputs):
    f = lambda a: np.ascontiguousarray(np.asarray(a, dtype=np.float32))
    wr = np.concatenate([f(inputs["router_group_w"])] + [f(inputs["router_expert_w"])[:, g] for g in range(4)], axis=2)
    br = np.concatenate([f(inputs["router_group_b"]), f(inputs["router_expert_b"]).reshape(DEPTH, 32)], axis=1)
    shared = {
        "w_ada": f(inputs["w_ada"]), "b_ada": f(inputs["b_ada"]), "w_in": f(inputs["w_in"]), "conv_w": f(inputs["conv_w"]),
        "dec_f": f(inputs["ret_decay_fwd"]), "dec_b": f(inputs["ret_decay_bwd"]), "sgu_w": f(inputs["sgu_w"]),
        "sgu_b": f(inputs["sgu_b"]), "w_out": f(inputs["w_out"]), "ln1_g": f(inputs["ln1_g"]), "ln1_b": f(inputs["ln1_b"]),
        "wr": f(wr), "br": f(br), "wg": f(inputs["moe_w_gate"]), "wu": f(inputs["moe_w_up"]), "wd": f(inputs["moe_w_down"]),
        "ln2_g": f(inputs["ln2_g"]), "ln2_b": f(inputs["ln2_b"]), "consts": _consts(),
    }
    x, c, ctx, c_ctx = f(inputs["x"]), f(inputs["c"]), f(inputs["ctx"]), f(inputs["c_ctx"])
    maps = []
    for b in range(8):
        cT = np.stack([c[b].reshape(8, 128).T, c_ctx.reshape(8, 128).T], axis=2).reshape(128, 16)
        d = dict(shared)
        d.update({"x": x[b], "ctx": ctx[b], "cT": f(cT)})
        maps.append(d)
    return maps


def kernel(**inputs):
    nc = KB().build()
    maps = make_in_maps(inputs)
    res = run_bass_kernel_spmd(nc, maps, core_ids=list(range(8)))
    return np.stack([np.asarray(r["out"]) for r in res.results], axis=0).astype(np.float32)
```
